# Optimizing a Trainium2 kernel written in Bass

```python
import math
import jax, jax.numpy as jnp
from jax import lax
import numpy as np


D_MODEL = 1024
BATCH = 2
SEQ = 8192
DEPTH = 2

SSM_GROUP = 16
SSM_STATE = 64
SSM_WIDTH = D_MODEL // 2
SSM_GROUPS = SSM_WIDTH // SSM_GROUP
HEAD_DIM = 64
N_HEADS = D_MODEL // HEAD_DIM
N_KV = N_HEADS // 4
HPG = N_HEADS // N_KV
ATTN_WIDTH = N_HEADS * HEAD_DIM
KV_WIDTH = N_KV * HEAD_DIM
CMP_BLOCK = 32
CMP_STRIDE = 16
CMP_HIDDEN = 4 * HEAD_DIM
SLC_BLOCK = 64
SLC_TOPK = 16
WINDOW = 512
Q_CHUNK = 64
FORCED_SCORE = 1e4
IN_WIDTH = ATTN_WIDTH + 6 * KV_WIDTH + 3 * N_HEADS + SSM_WIDTH + 2 * D_MODEL
N_GROUPS = 4
EXP_PER_GROUP = 8
N_EXPERTS = N_GROUPS * EXP_PER_GROUP
TOPK_IN_GROUP = 2
D_EXPERT = D_MODEL // 4
MOE_BLOCK = 128
EPS = 1e-6
NEG = -1e30

kernel_name = "hybrid_s5_nsa_hmoe_adaln"


def rms_norm(x, g):
    xf = x.astype(jnp.float32)
    y = xf * lax.rsqrt(jnp.mean(xf * xf, axis=-1, keepdims=True) + EPS)
    return (y * g.astype(jnp.float32)).astype(x.dtype)


def masked_softmax(s, mask):
    p = jax.nn.softmax(jnp.where(mask, s.astype(jnp.float32), NEG), axis=-1)
    return p * mask


def s5_mixer(u, lam_re, lam_im, log_dt, b_re, b_im, c_re, c_im, d_skip, w_glu):
    f32 = jnp.float32
    Bsz, L, _ = u.shape
    ug = u.astype(f32).reshape(Bsz, L, SSM_GROUPS, SSM_GROUP)
    dt = jnp.exp(log_dt.astype(f32))[:, None]
    lr, li = lam_re.astype(f32), lam_im.astype(f32)
    mag = jnp.exp(lr * dt)
    ab_re, ab_im = mag * jnp.cos(li * dt), mag * jnp.sin(li * dt)
    den = lr * lr + li * li
    nr = ab_re - 1.0
    cr = (nr * lr + ab_im * li) / den
    cim = (ab_im * lr - nr * li) / den
    br, bim = b_re.astype(f32), b_im.astype(f32)
    bb_re = cr[..., None] * br - cim[..., None] * bim
    bb_im = cr[..., None] * bim + cim[..., None] * br
    bu_re = jnp.einsum('blgh,gph->blgp', ug, bb_re)
    bu_im = jnp.einsum('blgh,gph->blgp', ug, bb_im)
    a_re = jnp.broadcast_to(ab_re, bu_re.shape)
    a_im = jnp.broadcast_to(ab_im, bu_im.shape)

    def combine(e1, e2):
        a1r, a1i, b1r, b1i = e1
        a2r, a2i, b2r, b2i = e2
        return (a2r * a1r - a2i * a1i, a2r * a1i + a2i * a1r,
                a2r * b1r - a2i * b1i + b2r, a2r * b1i + a2i * b1r + b2i)

    _, _, s_re, s_im = lax.associative_scan(combine, (a_re, a_im, bu_re, bu_im), axis=1)
    y = (jnp.einsum('blgp,ghp->blgh', s_re, c_re.astype(f32))
         - jnp.einsum('blgp,ghp->blgh', s_im, c_im.astype(f32))
         + d_skip.astype(f32) * ug)
    y = jax.nn.gelu(y.reshape(Bsz, L, SSM_WIDTH))
    y = y * jax.nn.sigmoid(y @ w_glu.astype(f32))
    return y.astype(u.dtype)


def compress_kv(kv, pos, w1, w2):
    Bsz, L, G, hd = kv.shape
    n_cmp = (L - CMP_BLOCK) // CMP_STRIDE + 1
    idx = jnp.arange(n_cmp)[:, None] * CMP_STRIDE + jnp.arange(CMP_BLOCK)[None, :]
    blocks = kv[:, idx] + pos[:, None, :]
    blocks = jnp.swapaxes(blocks, 2, 3).reshape(Bsz, n_cmp, G, CMP_BLOCK * hd)
    return jax.nn.gelu(blocks @ w1) @ w2


def nsa_attention(q, k_cmp, v_cmp, k_slc, v_slc, k_win, v_win, gates, cmp_pos, cmp_w1, cmp_w2):
    Bsz, L = q.shape[:2]
    n_cmp = (L - CMP_BLOCK) // CMP_STRIDE + 1
    n_slc = L // SLC_BLOCK
    n_sel = min(SLC_TOPK, n_slc)
    n_chunks = L // Q_CHUNK
    scale = HEAD_DIM ** -0.5
    kc = compress_kv(k_cmp, cmp_pos[0], cmp_w1[0], cmp_w2[0])
    vc = compress_kv(v_cmp, cmp_pos[1], cmp_w1[1], cmp_w2[1])
    cmp_end = jnp.arange(n_cmp) * CMP_STRIDE + CMP_BLOCK - 1
    c_start = jnp.arange(n_cmp)[:, None] * CMP_STRIDE
    s_start = jnp.arange(n_slc)[None, :] * SLC_BLOCK
    overlap = ((c_start < s_start + SLC_BLOCK) & (c_start + CMP_BLOCK > s_start)).astype(jnp.float32)
    ks_blk = jnp.moveaxis(k_slc.reshape(Bsz, n_slc, SLC_BLOCK, N_KV, HEAD_DIM), 3, 1)
    vs_blk = jnp.moveaxis(v_slc.reshape(Bsz, n_slc, SLC_BLOCK, N_KV, HEAD_DIM), 3, 1)
    kw = jnp.pad(k_win, ((0, 0), (WINDOW, 0), (0, 0), (0, 0)))
    vw = jnp.pad(v_win, ((0, 0), (WINDOW, 0), (0, 0), (0, 0)))
    bi = jnp.arange(Bsz)[:, None, None, None]
    gi = jnp.arange(N_KV)[None, :, None, None]
    blk = jnp.arange(n_slc)

    def chunk(args):
        c_idx, qc, gc = args
        t = c_idx * Q_CHUNK + jnp.arange(Q_CHUNK)
        s = jnp.einsum('bqghd,bngd->bqghn', qc, kc) * scale
        m = cmp_end[None, :] <= t[:, None]
        p_cmp = masked_softmax(s, m[None, :, None, None, :])
        o_cmp = jnp.einsum('bqghn,bngd->bqghd', p_cmp.astype(vc.dtype), vc)
        imp = jnp.einsum('bqghn,ns->bqgs', p_cmp, overlap)
        cur = t[:, None] // SLC_BLOCK
        forced = (blk[None, :] == 0) | (blk[None, :] == cur) | (blk[None, :] == cur - 1)
        future = blk[None, :] * SLC_BLOCK > t[:, None]
        imp = jnp.where(forced[None, :, None, :], FORCED_SCORE, imp)
        imp = jnp.where(future[None, :, None, :], -1.0, imp)
        _, sel = lax.top_k(imp, n_sel)
        sel = jnp.transpose(sel, (0, 2, 1, 3))
        ks = ks_blk[bi, gi, sel].reshape(Bsz, N_KV, Q_CHUNK, n_sel * SLC_BLOCK, HEAD_DIM)
        vs = vs_blk[bi, gi, sel].reshape(Bsz, N_KV, Q_CHUNK, n_sel * SLC_BLOCK, HEAD_DIM)
        kpos = (sel[..., None] * SLC_BLOCK + jnp.arange(SLC_BLOCK)).reshape(Bsz, N_KV, Q_CHUNK, n_sel * SLC_BLOCK)
        m = jnp.transpose(kpos <= t[None, None, :, None], (0, 2, 1, 3))[:, :, :, None, :]
        s = jnp.einsum('bqghd,bgqkd->bqghk', qc, ks) * scale
        p = masked_softmax(s, m)
        o_slc = jnp.einsum('bqghk,bgqkd->bqghd', p.astype(vs.dtype), vs)
        start = c_idx * Q_CHUNK
        kwc = lax.dynamic_slice_in_dim(kw, start, WINDOW + Q_CHUNK, axis=1)
        vwc = lax.dynamic_slice_in_dim(vw, start, WINDOW + Q_CHUNK, axis=1)
        wpos = start - WINDOW + jnp.arange(WINDOW + Q_CHUNK)
        d = t[:, None] - wpos[None, :]
        m = (d >= 0) & (d < WINDOW) & (wpos[None, :] >= 0)
        s = jnp.einsum('bqghd,bkgd->bqghk', qc, kwc) * scale
        p = masked_softmax(s, m[None, :, None, None, :])
        o_win = jnp.einsum('bqghk,bkgd->bqghd', p.astype(vwc.dtype), vwc)
        return (gc[:, :, 0, :, :, None] * o_cmp + gc[:, :, 1, :, :, None] * o_slc
                + gc[:, :, 2, :, :, None] * o_win)

    q_ch = jnp.moveaxis(q.reshape(Bsz, n_chunks, Q_CHUNK, N_KV, HPG, HEAD_DIM), 1, 0)
    g_ch = jnp.moveaxis(gates.reshape(Bsz, n_chunks, Q_CHUNK, 3, N_KV, HPG), 1, 0)
    o = lax.map(chunk, (jnp.arange(n_chunks), q_ch, g_ch))
    return jnp.moveaxis(o, 0, 1).reshape(Bsz, L, ATTN_WIDTH)


def token_mixer(h, w_in, ssm_lam_re, ssm_lam_im, ssm_log_dt, ssm_b_re, ssm_b_im, ssm_c_re, ssm_c_im,
                ssm_d, ssm_w_glu, ssm_w_o, nsa_cmp_pos, nsa_cmp_w1, nsa_cmp_w2, nsa_w_o, w_out):
    Bsz, L, _ = h.shape
    proj = h @ w_in
    o1 = ATTN_WIDTH
    o2 = o1 + 6 * KV_WIDTH
    o3 = o2 + 3 * N_HEADS
    o4 = o3 + SSM_WIDTH
    q, kv, nsa_g, u, merge_g = jnp.split(proj, [o1, o2, o3, o4], axis=-1)
    k_cmp, v_cmp, k_slc, v_slc, k_win, v_win = [t.reshape(Bsz, L, N_KV, HEAD_DIM) for t in jnp.split(kv, 6, axis=-1)]
    attn = nsa_attention(q.reshape(Bsz, L, N_KV, HPG, HEAD_DIM), k_cmp, v_cmp, k_slc, v_slc, k_win, v_win,
                         jax.nn.sigmoid(nsa_g).reshape(Bsz, L, 3, N_KV, HPG), nsa_cmp_pos, nsa_cmp_w1, nsa_cmp_w2)
    ssm = s5_mixer(u, ssm_lam_re, ssm_lam_im, ssm_log_dt, ssm_b_re, ssm_b_im, ssm_c_re, ssm_c_im, ssm_d, ssm_w_glu)
    g_attn, g_ssm = jnp.split(jax.nn.sigmoid(merge_g), 2, axis=-1)
    merged = g_attn * (attn @ nsa_w_o) + g_ssm * (ssm @ ssm_w_o)
    return merged @ w_out


def hier_moe(h, w_group, b_group, w_expert, b_expert, w_gate, w_up, w_down):
    Bsz, L, D = h.shape
    xt = h.reshape(-1, D)
    N = xt.shape[0]
    g_prob = jax.nn.softmax((xt @ w_group + b_group).astype(jnp.float32), axis=-1)
    g_p, g_idx = lax.top_k(g_prob, 1)
    e_logits = (xt @ w_expert + b_expert).astype(jnp.float32).reshape(N, N_GROUPS, EXP_PER_GROUP)
    e_logits = jnp.take_along_axis(e_logits, jnp.broadcast_to(g_idx[:, :, None], (N, 1, EXP_PER_GROUP)), axis=1)[:, 0]
    e_p, e_idx = lax.top_k(jax.nn.softmax(e_logits, axis=-1), TOPK_IN_GROUP)
    w = g_p * e_p / jnp.sum(e_p, axis=-1, keepdims=True)
    eid = g_idx * EXP_PER_GROUP + e_idx
    A = N * TOPK_IN_GROUP
    flat_e = eid.reshape(-1)
    flat_t = jnp.repeat(jnp.arange(N, dtype=jnp.int32), TOPK_IN_GROUP)
    flat_w = w.reshape(-1)
    order = jnp.argsort(flat_e)
    se = flat_e[order]
    counts = jnp.zeros(N_EXPERTS, jnp.int32).at[flat_e].add(1)
    starts = jnp.cumsum(counts) - counts
    pcounts = (counts + MOE_BLOCK - 1) // MOE_BLOCK * MOE_BLOCK
    pends = jnp.cumsum(pcounts)
    pstarts = pends - pcounts
    dest = pstarts[se] + jnp.arange(A) - starts[se]
    P = A + N_EXPERTS * MOE_BLOCK
    n_blk = P // MOE_BLOCK
    buf_t = jnp.zeros(P, jnp.int32).at[dest].set(flat_t[order])
    buf_w = jnp.zeros(P, jnp.float32).at[dest].set(flat_w[order])
    blk_e = jnp.minimum(jnp.searchsorted(pends, jnp.arange(n_blk) * MOE_BLOCK, side='right'), N_EXPERTS - 1)
    xs = xt[buf_t].reshape(n_blk, MOE_BLOCK, D)

    def expert_block(args):
        xb, e = args
        return (jax.nn.silu(xb @ w_gate[e]) * (xb @ w_up[e])) @ w_down[e]

    ys = lax.map(expert_block, (xs, blk_e)).reshape(P, D)
    out = jnp.zeros((N, D), jnp.float32).at[buf_t].add(ys.astype(jnp.float32) * buf_w[:, None])
    return out.astype(h.dtype).reshape(Bsz, L, D)


def setup_inputs(seed: int = 0) -> dict:
    key = jax.random.key(seed)
    ks = jax.random.split(key, 32)
    f32 = jnp.float32

    def nrm(k, shape, s):
        return jax.random.normal(k, shape, f32) * s

    n_idx = jnp.arange(SSM_STATE, dtype=f32)
    return {
        'x': nrm(ks[0], (BATCH, SEQ, D_MODEL), 1.0),
        'c': nrm(ks[1], (BATCH, D_MODEL), 1.0),
        'ada_w': nrm(ks[2], (DEPTH, D_MODEL, 6 * D_MODEL), 0.5 * D_MODEL ** -0.5),
        'ada_b': nrm(ks[3], (DEPTH, 6 * D_MODEL), 0.01),
        'norm1_g': 1.0 + nrm(ks[4], (DEPTH, D_MODEL), 0.02),
        'w_in': nrm(ks[5], (DEPTH, D_MODEL, IN_WIDTH), D_MODEL ** -0.5),
        'ssm_lam_re': -0.5 + nrm(ks[6], (DEPTH, SSM_GROUPS, SSM_STATE), 0.01),
        'ssm_lam_im': math.pi * n_idx + nrm(ks[7], (DEPTH, SSM_GROUPS, SSM_STATE), 0.01),
        'ssm_log_dt': jax.random.uniform(ks[8], (DEPTH, SSM_GROUPS), f32, math.log(1e-3), math.log(1e-1)),
        'ssm_b_re': nrm(ks[9], (DEPTH, SSM_GROUPS, SSM_STATE, SSM_GROUP), (2 * SSM_GROUP) ** -0.5),
        'ssm_b_im': nrm(ks[10], (DEPTH, SSM_GROUPS, SSM_STATE, SSM_GROUP), (2 * SSM_GROUP) ** -0.5),
        'ssm_c_re': nrm(ks[11], (DEPTH, SSM_GROUPS, SSM_GROUP, SSM_STATE), SSM_STATE ** -0.5),
        'ssm_c_im': nrm(ks[12], (DEPTH, SSM_GROUPS, SSM_GROUP, SSM_STATE), SSM_STATE ** -0.5),
        'ssm_d': nrm(ks[13], (DEPTH, SSM_GROUPS, SSM_GROUP), 1.0),
        'ssm_w_glu': nrm(ks[14], (DEPTH, SSM_WIDTH, SSM_WIDTH), SSM_WIDTH ** -0.5),
        'ssm_w_o': nrm(ks[15], (DEPTH, SSM_WIDTH, D_MODEL), SSM_WIDTH ** -0.5),
        'nsa_cmp_pos': nrm(ks[16], (DEPTH, 2, CMP_BLOCK, HEAD_DIM), 0.02),
        'nsa_cmp_w1': nrm(ks[17], (DEPTH, 2, CMP_BLOCK * HEAD_DIM, CMP_HIDDEN), (CMP_BLOCK * HEAD_DIM) ** -0.5),
        'nsa_cmp_w2': nrm(ks[18], (DEPTH, 2, CMP_HIDDEN, HEAD_DIM), CMP_HIDDEN ** -0.5),
        'nsa_w_o': nrm(ks[19], (DEPTH, ATTN_WIDTH, D_MODEL), ATTN_WIDTH ** -0.5),
        'w_out': nrm(ks[20], (DEPTH, D_MODEL, D_MODEL), D_MODEL ** -0.5),
        'norm2_g': 1.0 + nrm(ks[21], (DEPTH, D_MODEL), 0.02),
        'moe_w_group': nrm(ks[22], (DEPTH, D_MODEL, N_GROUPS), D_MODEL ** -0.5),
        'moe_b_group': nrm(ks[23], (DEPTH, N_GROUPS), 0.01),
        'moe_w_expert': nrm(ks[24], (DEPTH, D_MODEL, N_EXPERTS), D_MODEL ** -0.5),
        'moe_b_expert': nrm(ks[25], (DEPTH, N_EXPERTS), 0.01),
        'moe_w_gate': nrm(ks[26], (DEPTH, N_EXPERTS, D_MODEL, D_EXPERT), D_MODEL ** -0.5),
        'moe_w_up': nrm(ks[27], (DEPTH, N_EXPERTS, D_MODEL, D_EXPERT), D_MODEL ** -0.5),
        'moe_w_down': nrm(ks[28], (DEPTH, N_EXPERTS, D_EXPERT, D_MODEL), D_EXPERT ** -0.5),
        'final_g': 1.0 + nrm(ks[29], (D_MODEL,), 0.02),
    }


def reference(x, c, ada_w, ada_b, norm1_g, w_in, ssm_lam_re, ssm_lam_im, ssm_log_dt, ssm_b_re, ssm_b_im,
              ssm_c_re, ssm_c_im, ssm_d, ssm_w_glu, ssm_w_o, nsa_cmp_pos, nsa_cmp_w1, nsa_cmp_w2, nsa_w_o,
              w_out, norm2_g, moe_w_group, moe_b_group, moe_w_expert, moe_b_expert, moe_w_gate, moe_w_up,
              moe_w_down, final_g):
    cs = jax.nn.silu(c)
    for l in range(DEPTH):
        mod = cs @ ada_w[l] + ada_b[l]
        sh1, sc1, g1, sh2, sc2, g2 = jnp.split(mod[:, None, :], 6, axis=-1)
        h = rms_norm(x, norm1_g[l]) * (1.0 + sc1) + sh1
        x = x + g1 * token_mixer(h, w_in[l], ssm_lam_re[l], ssm_lam_im[l], ssm_log_dt[l], ssm_b_re[l],
                                 ssm_b_im[l], ssm_c_re[l], ssm_c_im[l], ssm_d[l], ssm_w_glu[l], ssm_w_o[l],
                                 nsa_cmp_pos[l], nsa_cmp_w1[l], nsa_cmp_w2[l], nsa_w_o[l], w_out[l])
        h = rms_norm(x, norm2_g[l]) * (1.0 + sc2) + sh2
        x = x + g2 * hier_moe(h, moe_w_group[l], moe_b_group[l], moe_w_expert[l], moe_b_expert[l],
                              moe_w_gate[l], moe_w_up[l], moe_w_down[l])
    return rms_norm(x, final_g)
```

```python
import contextlib
import numpy as np
import concourse.bass as bass
import concourse.mybir as mybir
from concourse.bass_utils import run_bass_kernel_spmd

F32 = mybir.dt.float32
I32 = mybir.dt.int32
AF = mybir.ActivationFunctionType
ALU = mybir.AluOpType
AX = mybir.AxisListType

D = 1024
SEQ = 8192
NEGM = -1.0e5
EPS = 1e-6
TWO_PI = float(2 * np.pi)


class Sch:
    EPOCH = 30000
    NDMA = 24

    def __init__(self, nc):
        self.nc = nc
        self.eng = {'pe': nc.tensor, 'dve': nc.vector, 'act': nc.scalar, 'pool': nc.gpsimd, 'sp': nc.sync}
        self.sem = {}
        self.cnt = {}
        self.nsem = 0
        for k in self.eng:
            self._new_sem(k)
        self.seen = {k: {} for k in self.eng}
        self.dma = []
        for i in range(self.NDMA):
            self.dma.append([nc.alloc_semaphore(name=f"dma{i}"), 0, f"dma{i}"])
        self.dma_rr = 0
        self.last_w = {}
        self.readers = {}
        self.out_tokens = []
        self.n_inst = 0

    def _new_sem(self, k):
        self.nsem += 1
        self.sem[k] = (self.nc.alloc_semaphore(name=f"e_{k}_{self.nsem}"), f"e_{k}_{self.nsem}")
        self.cnt[k] = 0

    def _wait(self, e, tok):
        h, sid, val, owner = tok
        if owner == 'pe' and e == 'pe':
            return
        if self.seen[e].get(sid, 0) >= val:
            return
        self.eng[e].wait_ge(h, val)
        self.seen[e][sid] = val
        self.n_inst += 1

    def _deps(self, e, reads, writes):
        toks = []
        for b in reads:
            t = self.last_w.get(b)
            if t is not None:
                toks.append(t)
        for b in writes:
            t = self.last_w.get(b)
            if t is not None:
                toks.append(t)
            toks.extend(self.readers.get(b, ()))
        for t in toks:
            self._wait(e, t)

    def _commit(self, tok, reads, writes):
        for b in reads:
            if b not in writes:
                self.readers.setdefault(b, []).append(tok)
        for b in writes:
            self.last_w[b] = tok
            self.readers[b] = []

    def op(self, e, fn, reads=(), writes=()):
        self._deps(e, reads, writes)
        if self.cnt[e] >= self.EPOCH:
            self._new_sem(e)
        inst = fn(self.eng[e])
        h, sid = self.sem[e]
        self.cnt[e] += 1
        inst.then_inc(h, 1)
        tok = (h, sid, self.cnt[e], e)
        self._commit(tok, reads, writes)
        self.n_inst += 1
        return tok

    def dma(self_, *a, **k):
        raise NotImplementedError

    def dma_start(self, e, out, in_, reads=(), writes=(), is_output=False, **kw):
        self._deps(e, reads, writes)
        slot = self.dma[self.dma_rr]
        self.dma_rr = (self.dma_rr + 1) % self.NDMA
        h, val, sid = slot
        if val > 0:
            self._wait(e, (h, sid, val, 'dma'))
        inst = self.eng[e].dma_start(out=out, in_=in_, **kw)
        slot[1] = val + 16
        inst.then_inc(h, 16)
        tok = (h, sid, val + 16, 'dma')
        self._commit(tok, reads, writes)
        if is_output:
            self.out_tokens.append(tok)
        self.n_inst += 1
        return tok

    def finish(self, e='sp'):
        for t in self.out_tokens:
            self._wait(e, t)
        for k in self.eng:
            if k != e and self.cnt[k] > 0:
                h, sid = self.sem[k]
                self._wait(e, (h, sid, self.cnt[k], k))


def _const_tables():
    q = np.arange(128)[:, None]
    k = np.arange(128)[None, :]
    triq = np.where(k <= q, 0.0, NEGM).astype(np.float32)
    tri2q = np.where(k > q, 0.0, NEGM).astype(np.float32)
    ident = np.eye(128, dtype=np.float32)
    id4 = np.tile(ident, (1, 4))
    keep = np.ones((128, 256), np.float32)
    add = np.zeros((128, 256), np.float32)
    for qq in range(128):
        half = qq // 64
        for c in range(256):
            rel = c - 128
            if rel > half:
                keep[qq, c] = 0.0; add[qq, c] = -1.0
            elif rel == half:
                keep[qq, c] = 0.0; add[qq, c] = 1.0e4
            elif rel == half - 1:
                keep[qq, c] = 0.0; add[qq, c] = 2.0e4
    ov = np.zeros((512, 128), np.float32)
    for m in range(1, 512):
        n = m - 1
        for s in range(128):
            if 16 * n < 64 * s + 64 and 16 * n + 32 > 64 * s:
                ov[m, s] = 1.0
    tabs = []
    tab_index = {}
    keymap = {}
    for i in range(64):
        nb = (8 * i + 7) // 128 + 1
        for jb in range(nb):
            if jb >= 1 and 128 * jb + 127 <= 8 * i - 1:
                continue
            m = 128 * jb + np.arange(128)[None, :]
            t = 128 * i + np.arange(128)[:, None]
            vis = (m >= 1) & (16 * (m - 1) + 31 <= t)
            tab = np.where(vis, 0.0, NEGM).astype(np.float32)
            key = tab.tobytes()
            if key not in tab_index:
                tab_index[key] = len(tabs)
                tabs.append(tab)
            keymap[(i, jb)] = tab_index[key]
    return dict(triq=triq, tri2q=tri2q, ident=ident, id4=id4, keep=keep, add=add,
                ov=ov.reshape(4, 128, 128).transpose(1, 0, 2).copy(), cm=np.stack(tabs)), keymap


_CONST, _CMKEY = _const_tables()
NQ = 256
NCOL = 908
C_Q = 0
C_KC = 256
C_VC = 384
C_KS = 512
C_KW = 576
C_TM = 640
C_U = 780


def _sel_cols(g):
    o1 = 1024
    o2 = o1 + 6 * 256
    o3 = o2 + 48
    cols = list(range(g * 256, g * 256 + 256))
    kc = list(range(o1 + 0 * 256 + g * 64, o1 + 0 * 256 + g * 64 + 64))
    vc = list(range(o1 + 1 * 256 + g * 64, o1 + 1 * 256 + g * 64 + 64))
    ks = list(range(o1 + 2 * 256 + g * 64, o1 + 2 * 256 + g * 64 + 64))
    vs = list(range(o1 + 3 * 256 + g * 64, o1 + 3 * 256 + g * 64 + 64))
    kw = list(range(o1 + 4 * 256 + g * 64, o1 + 4 * 256 + g * 64 + 64))
    vw = list(range(o1 + 5 * 256 + g * 64, o1 + 5 * 256 + g * 64 + 64))
    gt = [o2 + br * 16 + g * 4 + h for br in range(3) for h in range(4)]
    u = list(range(o3 + g * 128, o3 + g * 128 + 128))
    cols = cols + kc + kc + vc + vc + ks + kw + vs + vw + gt + u
    assert len(cols) == NCOL
    return np.array(cols)


def build_A(n_tiles=SEQ // 512, debug=False):
    nc = bass.Bass("TRN2", target_bir_lowering=False)

    def din(name, shape):
        return nc.dram_tensor(name, list(shape), F32, kind="ExternalInput").ap()

    xT = din("xT", [8, 128, SEQ])
    cst = din("cst", [128, 8])
    adaw = din("adaw", [16, 128, 8, 128])
    adab = din("adab", [128, 16])
    n1g = din("n1g", [128, 8])
    wsel = din("wsel", [128, 8, NCOL])
    w1d = din("w1d", [4, 128, 16, 128])
    pos2 = din("pos2", [128, 2, 16])
    w2d = din("w2d", [128, 2, 2, 64])
    c_triq = din("triq", [128, 128]); c_tri2q = din("tri2q", [128, 128])
    c_ident = din("ident", [128, 128]); c_id4 = din("id4", [128, 512])
    c_keep = din("keep", [128, 256]); c_add = din("add", [128, 256])
    c_ov = din("ov", [128, 4, 128])
    c_cm = din("cm", list(_CONST['cm'].shape))
    s_lr = din("s_lr", [128, 512]); s_li = din("s_li", [128, 512]); s_ldt = din("s_ldt", [128, 512])
    s_bre = din("s_bre", [128, 512]); s_bim = din("s_bim", [128, 512])
    s_lrc = din("s_lrc", [128, 4]); s_lic = din("s_lic", [128, 4]); s_ldtc = din("s_ldtc", [128, 4])
    s_cre = din("s_cre", [128, 4, 128]); s_cim = din("s_cim", [128, 4, 128])
    s_d = din("s_d", [128, 1])
    s_iota = din("s_iota", [128, 512])
    o_attn = nc.dram_tensor("o_attn", [SEQ, 256], F32, kind="ExternalOutput").ap()
    o_ssm = nc.dram_tensor("o_ssm", [128, SEQ], F32, kind="ExternalOutput").ap()
    o_dbg = nc.dram_tensor("o_dbg", [64, 128, 4, 128], F32, kind="ExternalOutput").ap() if debug else None

    S = Sch(nc)
    with contextlib.ExitStack() as st:
        def sb(name, shape, dt=F32):
            return st.enter_context(nc.sbuf_tensor(name, list(shape), dt))

        def ps(name, shape):
            return st.enter_context(nc.psum_tensor(name, list(shape), F32))

        W = sb("W", [128, 8, NCOL])
        biasrow = sb("biasrow", [1, NCOL])
        ones = sb("ones", [128, 128]); zer = sb("zer", [128, 512])
        triq = sb("triq_s", [128, 128]); tri2q = sb("tri2q_s", [128, 128])
        ident = sb("ident_s", [128, 128]); id4 = sb("id4_s", [128, 512])
        keep = sb("keep_s", [128, 256]); addt = sb("add_s", [128, 256]); ov = sb("ov_s", [128, 4, 128])
        w1s = [sb(f"w1s{i}", [128, 16, 128]) for i in range(1)]
        w2s = sb("w2s", [128, 2, 2, 64]); b1 = sb("b1", [128, 4]); pos2s = sb("pos2s", [128, 2, 16])
        BBre = sb("BBre", [128, 512]); BBim = sb("BBim", [128, 512])
        CCre = sb("CCre", [128, 4, 128]); CCim = sb("CCim", [128, 4, 128])
        cosT = sb("cosT", [128, 4, 256]); sinT = sb("sinT", [128, 4, 256])
        rcolS = sb("rcolS", [128, 4]); thc = sb("thc", [128, 4]); dcol = sb("dcol", [128, 1])
        carry_re = sb("carry_re", [128, 4]); carry_im = sb("carry_im", [128, 4])
        erc = sb("erc", [128, 4]); eic = sb("eic", [128, 4])

        pA = [ps(f"pA{i}", [128, 512]) for i in range(2)]
        pS = [ps(f"pS{i}", [128, 512]) for i in range(3)]
        pOf = [ps(f"pO{i}", [128, 512]) for i in range(2)]
        pO = [t_[:, 0:260].rearrange("p (a b) -> p a b", a=4) for t_ in pOf]
        pI = ps("pI", [128, 4, 128])
        rot = {'pA': 0, 'pS': 0, 'pO': 0, 'pT': 0, 'xsq': 0, 'ne': 0, 'cmt': 0, 'w1s': 0, 'aout': 0, 'yT': 0}

        def nxt(k, n=2):
            v = rot[k]; rot[k] = (v + 1) % n
            return v

        def mm(out, lhsT, rhs, start, stop, reads, writes, skip=False):
            if skip:
                S.op('pe', lambda e: e.matmul(out, lhsT=lhsT, rhs=rhs, start=start, stop=stop, skip_group_check=True), reads=reads, writes=writes)
            else:
                S.op('pe', lambda e: e.matmul(out, lhsT=lhsT, rhs=rhs, start=start, stop=stop), reads=reads, writes=writes)

        S.dma_start('sp', W[:], wsel, writes=['W'])
        for nm, t, src in (('triq', triq, c_triq), ('tri2q', tri2q, c_tri2q), ('ident', ident, c_ident), ('id4', id4, c_id4),
                           ('keep', keep, c_keep), ('add', addt, c_add), ('ov', ov, c_ov), ('w2s', w2s, w2d), ('pos2s', pos2s, pos2)):
            S.dma_start('sp', t[:], src, writes=[nm])
        S.op('pool', lambda e: e.memset(ones[:], 1.0), writes=['ones'])
        S.op('pool', lambda e: e.memset(zer[:], 0.0), writes=['zer'])
        S.op('pool', lambda e: e.memset(carry_re[:], 0.0), writes=['carry'])
        S.op('pool', lambda e: e.memset(carry_im[:], 0.0), writes=['carry'])

        with contextlib.ExitStack() as st2:
            def sb2(name, shape, dt=F32):
                return st2.enter_context(nc.sbuf_tensor(name, list(shape), dt))
            cs = sb2("cs", [128, 8]); sg = sb2("sg", [128, 8])
            adat = [sb2(f"adat{i}", [128, 8, 128]) for i in range(2)]
            adabs = sb2("adabs", [128, 16]); modc = sb2("modc", [128, 16]); n1 = sb2("n1", [128, 8]); scl = sb2("scl", [128, 8])
            S.dma_start('sp', cs[:], cst, writes=['cs'])
            S.dma_start('sp', adabs[:], adab, writes=['adabs'])
            S.dma_start('sp', n1[:], n1g, writes=['n1'])
            S.op('act', lambda e: e.activation(out=sg[:], in_=cs[:], func=AF.Sigmoid), reads=['cs'], writes=['sg'])
            S.op('dve', lambda e: e.tensor_tensor(out=cs[:], in0=cs[:], in1=sg[:], op=ALU.mult), reads=['cs', 'sg'], writes=['cs'])
            for j in range(16):
                a = adat[j % 2]; an = f"adat{j % 2}"
                S.dma_start('sp', a[:], adaw[j], writes=[an])
                p = pA[nxt('pA')]; pn = f"pA{rot['pA'] ^ 1}"
                for kc in range(8):
                    mm(p[:, 0:1], a[:, kc, :], cs[:, kc:kc + 1], kc == 0, kc == 7, [an, 'cs'], [pn])
                S.op('dve', lambda e: e.tensor_tensor(out=modc[:, j:j + 1], in0=p[:, 0:1], in1=adabs[:, j:j + 1], op=ALU.add),
                     reads=[pn, 'adabs'], writes=['modc'])
            for (c0, c1) in ((0, 512), (512, NCOL)):
                p = pA[nxt('pA')]; pn = f"pA{rot['pA'] ^ 1}"
                for kc in range(8):
                    mm(p[0:1, 0:c1 - c0], modc[:, kc:kc + 1], W[:, kc, c0:c1], kc == 0, kc == 7, ['modc', 'W'], [pn])
                S.op('act', lambda e: e.activation(out=biasrow[0:1, c0:c1], in_=p[0:1, 0:c1 - c0], func=AF.Copy), reads=[pn], writes=['biasrow'])
            S.op('dve', lambda e: e.tensor_scalar(out=scl[:], in0=modc[:, 8:16], scalar1=1.0, scalar2=None, op0=ALU.add), reads=['modc'], writes=['scl'])
            S.op('dve', lambda e: e.tensor_tensor(out=scl[:], in0=scl[:], in1=n1[:], op=ALU.mult), reads=['scl', 'n1'], writes=['scl'])
            for kc in range(8):
                S.op('dve' if kc % 2 else 'pool', lambda e: e.tensor_scalar(out=W[:, kc, :], in0=W[:, kc, :], scalar1=scl[:, kc:kc + 1], scalar2=None, op0=ALU.mult),
                     reads=['W', 'scl'], writes=['W'])
            for c4 in range(4):
                kv = c4 // 2
                w = w1s[0]; wn = "w1s0"
                S.dma_start('sp', w[:], w1d[c4], writes=[wn])
                p = pA[nxt('pA')]; pn = f"pA{rot['pA'] ^ 1}"
                for lp in range(16):
                    mm(p[:, 0:1], w[:, lp, :], pos2s[:, kv, lp:lp + 1], lp == 0, lp == 15, [wn, 'pos2s'], [pn])
                S.op('act', lambda e: e.activation(out=b1[:, c4:c4 + 1], in_=p[:, 0:1], func=AF.Copy), reads=[pn], writes=['b1'])

            t = [sb2(f"pt{i}", [128, 512]) for i in range(10)]
            ti = sb2("pti", [128, 512], I32)
            for nm, tt, src in (('t0', t[0], s_lr), ('t1', t[1], s_li), ('t2', t[2], s_ldt), ('t8', t[8], s_bre), ('t9', t[9], s_bim)):
                S.dma_start('sp', tt[:], src, writes=[nm])
            S.dma_start('sp', CCre[:], s_cre, writes=['CC']); S.dma_start('sp', CCim[:], s_cim, writes=['CC'])
            S.dma_start('sp', dcol[:], s_d, writes=['dcol'])

            def dv(fn, reads, writes, e='dve'):
                S.op(e, fn, reads=reads, writes=writes)

            def sincos(out_sin, out_cos, arg, n, names, onames):
                for (o, shift) in ((out_sin, 0.5), (out_cos, 0.75)):
                    dv(lambda e: e.tensor_scalar(out=t[6][:, 0:n], in0=arg, scalar1=float(1 / TWO_PI), scalar2=shift, op0=ALU.mult, op1=ALU.add), names, ['t6'])
                    dv(lambda e: e.tensor_copy(out=ti[:, 0:n], in_=t[6][:, 0:n]), ['t6'], ['ti'])
                    dv(lambda e: e.tensor_copy(out=t[7][:, 0:n], in_=ti[:, 0:n]), ['ti'], ['t7'])
                    dv(lambda e: e.tensor_tensor(out=t[6][:, 0:n], in0=t[6][:, 0:n], in1=t[7][:, 0:n], op=ALU.subtract), ['t6', 't7'], ['t6'])
                    dv(lambda e: e.tensor_scalar(out=t[7][:, 0:n], in0=t[6][:, 0:n], scalar1=0.0, scalar2=None, op0=ALU.is_lt), ['t6'], ['t7'])
                    dv(lambda e: e.tensor_tensor(out=t[6][:, 0:n], in0=t[6][:, 0:n], in1=t[7][:, 0:n], op=ALU.add), ['t6', 't7'], ['t6'])
                    dv(lambda e: e.tensor_scalar(out=t[6][:, 0:n], in0=t[6][:, 0:n], scalar1=TWO_PI, scalar2=float(np.pi), op0=ALU.mult, op1=ALU.subtract), ['t6'], ['t6'])
                    dv(lambda e: e.tensor_scalar(out=t[6][:, 0:n], in0=t[6][:, 0:n], scalar1=float(np.pi), scalar2=float(-np.pi), op0=ALU.min, op1=ALU.max), ['t6'], ['t6'])
                    S.op('act', lambda e: e.activation(out=o, in_=t[6][:, 0:n], func=AF.Sin), reads=['t6'], writes=onames + ['trig'])

            S.op('act', lambda e: e.activation(out=t[2][:], in_=t[2][:], func=AF.Exp), reads=['t2'], writes=['t2'])
            dv(lambda e: e.tensor_tensor(out=t[3][:], in0=t[0][:], in1=t[2][:], op=ALU.mult), ['t0', 't2'], ['t3'])
            S.op('act', lambda e: e.activation(out=t[3][:], in_=t[3][:], func=AF.Exp), reads=['t3'], writes=['t3'])
            dv(lambda e: e.tensor_tensor(out=t[4][:], in0=t[1][:], in1=t[2][:], op=ALU.mult), ['t1', 't2'], ['t4'])
            sincos(t[5][:], t[2][:], t[4][:], 512, ['t4'], ['t5', 't2'])
            dv(lambda e: e.tensor_tensor(out=t[5][:], in0=t[5][:], in1=t[3][:], op=ALU.mult), ['t5', 't3', 'trig'], ['t5'])
            dv(lambda e: e.tensor_tensor(out=t[2][:], in0=t[2][:], in1=t[3][:], op=ALU.mult), ['t2', 't3', 'trig'], ['t2'])
            dv(lambda e: e.tensor_scalar(out=t[2][:], in0=t[2][:], scalar1=-1.0, scalar2=None, op0=ALU.add), ['t2'], ['t2'])
            dv(lambda e: e.tensor_tensor(out=t[3][:], in0=t[0][:], in1=t[0][:], op=ALU.mult), ['t0'], ['t3'])
            dv(lambda e: e.tensor_tensor(out=t[4][:], in0=t[1][:], in1=t[1][:], op=ALU.mult), ['t1'], ['t4'])
            dv(lambda e: e.tensor_tensor(out=t[3][:], in0=t[3][:], in1=t[4][:], op=ALU.add), ['t3', 't4'], ['t3'])
            dv(lambda e: e.reciprocal(out=t[3][:], in_=t[3][:]), ['t3'], ['t3'])
            dv(lambda e: e.tensor_tensor(out=t[6][:], in0=t[2][:], in1=t[0][:], op=ALU.mult), ['t2', 't0', 'trig'], ['t6'])
            dv(lambda e: e.tensor_tensor(out=t[4][:], in0=t[5][:], in1=t[1][:], op=ALU.mult), ['t5', 't1'], ['t4'])
            dv(lambda e: e.tensor_tensor(out=t[6][:], in0=t[6][:], in1=t[4][:], op=ALU.add), ['t6', 't4'], ['t6'])
            dv(lambda e: e.tensor_tensor(out=t[6][:], in0=t[6][:], in1=t[3][:], op=ALU.mult), ['t6', 't3'], ['t6'])
            dv(lambda e: e.tensor_tensor(out=t[7][:], in0=t[5][:], in1=t[0][:], op=ALU.mult), ['t5', 't0'], ['t7'])
            dv(lambda e: e.tensor_tensor(out=t[4][:], in0=t[2][:], in1=t[1][:], op=ALU.mult), ['t2', 't1'], ['t4'])
            dv(lambda e: e.tensor_tensor(out=t[7][:], in0=t[7][:], in1=t[4][:], op=ALU.subtract), ['t7', 't4'], ['t7'])
            dv(lambda e: e.tensor_tensor(out=t[7][:], in0=t[7][:], in1=t[3][:], op=ALU.mult), ['t7', 't3'], ['t7'])
            dv(lambda e: e.tensor_tensor(out=BBre[:], in0=t[6][:], in1=t[8][:], op=ALU.mult), ['t6', 't8'], ['BB'])
            dv(lambda e: e.tensor_tensor(out=t[4][:], in0=t[7][:], in1=t[9][:], op=ALU.mult), ['t7', 't9'], ['t4'])
            dv(lambda e: e.tensor_tensor(out=BBre[:], in0=BBre[:], in1=t[4][:], op=ALU.subtract), ['BB', 't4'], ['BB'])
            dv(lambda e: e.tensor_tensor(out=BBim[:], in0=t[6][:], in1=t[9][:], op=ALU.mult), ['t6', 't9'], ['BB'])
            dv(lambda e: e.tensor_tensor(out=t[4][:], in0=t[7][:], in1=t[8][:], op=ALU.mult), ['t7', 't8'], ['t4'])
            dv(lambda e: e.tensor_tensor(out=BBim[:], in0=BBim[:], in1=t[4][:], op=ALU.add), ['BB', 't4'], ['BB'])
            dv(lambda e: e.tensor_scalar(out=CCim[:], in0=CCim[:], scalar1=-1.0, scalar2=None, op0=ALU.mult), ['CC'], ['CC'])
            lrc = sb2("lrc", [128, 4]); lic = sb2("lic", [128, 4]); dtc = sb2("dtc", [128, 4])
            S.dma_start('sp', lrc[:], s_lrc, writes=['lrc']); S.dma_start('sp', lic[:], s_lic, writes=['lic']); S.dma_start('sp', dtc[:], s_ldtc, writes=['dtc'])
            S.op('act', lambda e: e.activation(out=dtc[:], in_=dtc[:], func=AF.Exp), reads=['dtc'], writes=['dtc'])
            dv(lambda e: e.tensor_tensor(out=rcolS[:], in0=lrc[:], in1=dtc[:], op=ALU.mult), ['lrc', 'dtc'], ['rcolS'])
            S.op('act', lambda e: e.activation(out=rcolS[:], in_=rcolS[:], func=AF.Exp), reads=['rcolS'], writes=['rcolS'])
            dv(lambda e: e.tensor_tensor(out=thc[:], in0=lic[:], in1=dtc[:], op=ALU.mult), ['lic', 'dtc'], ['thc'])
            sincos(eic[:], erc[:], thc[:], 4, ['thc'], ['eirc'])
            S.dma_start('sp', t[0][:], s_iota, writes=['t0'])
            for j in range(4):
                dv(lambda e: e.tensor_scalar(out=t[1][:, 0:256], in0=t[0][:, 0:256], scalar1=thc[:, j:j + 1], scalar2=None, op0=ALU.mult), ['t0', 'thc'], ['t1'])
                sincos(sinT[:, j, :], cosT[:, j, :], t[1][:, 0:256], 256, ['t1'], ['cossin'])
        _barrier(S)
        KT = sb("KT", [64, SEQ])
        kwr = sb("kwr", [64, 1024])
        vslc = sb("vslc", [128, 64, 65])
        vwr = sb("vwr", [128, 8, 65])
        kbuf = sb("kbuf", [128, 528]); vbuf = sb("vbuf", [128, 528])
        kcT = sb("kcT", [64, 512]); vc = sb("vc", [128, 4, 65])
        qT = sb("qT", [64, 4, 4, 128])
        uT = sb("uT", [128, 512])
        gts = sb("gts", [128, 4, 12])
        rcol = sb("rcol", [128, 4])
        xt = sb("xt", [128, 8, 512])
        xsq = [sb(f"xsq{i}", [128, 512]) for i in range(2)]
        sqbc = sb("sqbc", [128, 512]); rbc = sb("rbc", [128, 512])
        hidk = sb("hidk", [128, 2, 32]); hidv = sb("hidv", [128, 2, 128])
        pT = [sb(f"pT{i}", [128, 512]) for i in range(2)]
        negexp = [sb(f"negexp{i}", [128, 128]) for i in range(2)]
        cmt = [sb(f"cmt{i}", [128, 128]) for i in range(2)]
        imp = sb("imp", [128, 128]); imp2 = sb("imp2", [128, 128]); wk = sb("wk", [128, 128]); negm = sb("negm", [128, 128])
        m8 = sb("m8", [128, 8]); m8b = sb("m8b", [128, 8])
        den = sb("den", [128, 3, 4]); coef = sb("coef", [128, 3, 4])
        osb = [sb(f"osb{i}", [128, 4, 65]) for i in range(3)]
        aout = [sb(f"aout{i}", [128, 256]) for i in range(2)]
        cin_re = sb("cin_re", [128, 1]); cin_im = sb("cin_im", [128, 1]); ctmp = sb("ctmp", [128, 2])
        sw = [sb(f"sw{i}", [128, 256]) for i in range(8)]
        hre = sb("hre", [128, 4, 512]); him = sb("him", [128, 4, 512])
        yT = [sb(f"yT{i}", [128, 512]) for i in range(2)]

        S.op('pool', lambda e: e.memset(vslc[:], 1.0), writes=['vslc'])
        S.op('pool', lambda e: e.memset(vwr[:], 1.0), writes=['vwr'])
        S.op('pool', lambda e: e.memset(vc[:], 0.0), writes=['vc'])
        S.op('pool', lambda e: e.memset(vc[:, :, 64:65], 1.0), writes=['vc'])
        S.op('pool', lambda e: e.memset(kbuf[:], 0.0), writes=['kbuf'])
        S.op('pool', lambda e: e.memset(vbuf[:], 0.0), writes=['vbuf'])
        S.op('pool', lambda e: e.memset(kcT[:], 0.0), writes=['kcT'])

        _barrier(S)
        TRIG = ['trig']

        def evac_mul(dst, src, rb, reads, writes, e='dve'):
            S.op(e, lambda en: en.tensor_tensor(out=dst, in0=src, in1=rb, op=ALU.mult), reads=reads, writes=writes)

        pending_fin = []
        for I in range(n_tiles):
            t0 = I * 512
            S.dma_start('sp', xt[:], xT[:, :, t0:t0 + 512].rearrange("k p t -> p k t"), writes=['xt'])
            pq = pA[nxt('pA')]; pqn = f"pA{rot['pA'] ^ 1}"
            for kc in range(8):
                xi = nxt('xsq'); xs = xsq[xi]; xn = f"xsq{xi}"
                S.op('act', lambda e: e.activation(out=xs[:], in_=xt[:, kc, :], func=AF.Square), reads=['xt'], writes=[xn])
                mm(pq[:], ones[:], xs[:], kc == 0, kc == 7, ['ones', xn], [pqn])
            S.op('act', lambda e: e.activation(out=sqbc[:], in_=pq[:], func=AF.Sqrt, scale=1.0 / D, bias=EPS), reads=[pqn], writes=['sqbc'])
            S.op('dve', lambda e: e.reciprocal(out=rbc[:], in_=sqbc[:]), reads=['sqbc'], writes=['rbc'])
            pr = pA[nxt('pA')]; prn = f"pA{rot['pA'] ^ 1}"
            for ts in range(4):
                mm(pr[:, ts:ts + 1], rbc[0:1, ts * 128:(ts + 1) * 128], ones[0:1, 0:1], True, True, ['rbc', 'ones'], [prn])
            S.op('act', lambda e: e.activation(out=rcol[:], in_=pr[:, 0:4], func=AF.Copy), reads=[prn], writes=['rcol'])

            def fm(c0, M):
                p = pA[nxt('pA')]; pn = f"pA{rot['pA'] ^ 1}"
                for kc in range(8):
                    mm(p[0:M, :], W[:, kc, c0:c0 + M], xt[:, kc, :], kc == 0, False, ['W', 'xt'], [pn])
                mm(p[0:M, :], biasrow[0:1, c0:c0 + M], sqbc[0:1, :], False, True, ['biasrow', 'sqbc'], [pn])
                return p, pn
            for h in range(4):
                p, pn = fm(C_Q + 64 * h, 64)
                S.op('dve', lambda e: e.tensor_tensor(out=qT[:, :, h, :], in0=p[0:64, :].rearrange("p (a b) -> p a b", a=4),
                                                      in1=rbc[0:64, :].rearrange("p (a b) -> p a b", a=4), op=ALU.mult),
                     reads=[pn, 'rbc'], writes=['qT'])
            for (buf, bn, c0) in ((kbuf, 'kbuf', C_KC), (vbuf, 'vbuf', C_VC)):
                S.op('pool', lambda e: e.tensor_copy(out=buf[:, 0:16], in_=buf[:, 512:528]), reads=[bn], writes=[bn])
                p, pn = fm(c0, 128)
                evac_mul(buf[0:64, 16:528], p[0:64, :], rbc[0:64, :], [pn, 'rbc'], [bn])
                evac_mul(buf[64:128, 15:527], p[64:128, :], rbc[64:128, :], [pn, 'rbc'], [bn])
            p, pn = fm(C_KS, 64)
            evac_mul(KT[:, t0:t0 + 512], p[0:64, :], rbc[0:64, :], [pn, 'rbc'], [f'KT{I}'])
            p, pn = fm(C_KW, 64)
            r0 = (I % 2) * 512
            evac_mul(kwr[:, r0:r0 + 512], p[0:64, :], rbc[0:64, :], [pn, 'rbc'], [f'kwr{I % 2}'])
            p, pn = fm(C_U, 128)
            evac_mul(uT[:], p[:], rbc[:], [pn, 'rbc'], ['uT'])
            for ts in range(4):
                blk = I * 4 + ts
                p = pA[nxt('pA')]; pn = f"pA{rot['pA'] ^ 1}"
                for kc in range(8):
                    mm(p[:, 0:140], xt[:, kc, ts * 128:(ts + 1) * 128], W[:, kc, C_TM:C_TM + 140], kc == 0, False, ['xt', 'W'], [pn])
                mm(p[:, 0:140], sqbc[0:1, ts * 128:(ts + 1) * 128], biasrow[0:1, C_TM:C_TM + 140], False, True, ['sqbc', 'biasrow'], [pn])
                S.op('dve', lambda e: e.tensor_scalar(out=vslc[:, blk, 0:64], in0=p[:, 0:64], scalar1=rcol[:, ts:ts + 1], scalar2=None, op0=ALU.mult),
                     reads=[pn, 'rcol'], writes=[f'vslc{blk}'])
                S.op('dve', lambda e: e.tensor_scalar(out=vwr[:, blk % 8, 0:64], in0=p[:, 64:128], scalar1=rcol[:, ts:ts + 1], scalar2=None, op0=ALU.mult),
                     reads=[pn, 'rcol'], writes=[f'vwr{blk % 8}'])
                S.op('act', lambda e: e.activation(out=gts[:, ts, :], in_=p[:, 128:140], func=AF.Sigmoid, scale=rcol[:, ts:ts + 1]),
                     reads=[pn, 'rcol'], writes=['gts'])

            sl0 = 32 * I
            jb_new = sl0 // 128
            pc0 = sl0 % 128
            S.op('pool', lambda e: e.memset(hidv[:], 0.0), writes=['hidv'])
            for c4 in range(4):
                kv, hc = c4 // 2, c4 % 2
                wi = 0; w = w1s[wi]; wn = f"w1s{wi}"
                S.dma_start('sp', w[:], w1d[c4], writes=[wn])
                buf, bn = (kbuf, 'kbuf') if kv == 0 else (vbuf, 'vbuf')
                p = pA[nxt('pA')]; pn = f"pA{rot['pA'] ^ 1}"
                for lp in range(16):
                    mm(p[:, 0:32], w[:, lp, :], buf[:, 2 * lp:2 * lp + 16 * 31 + 1:16], lp == 0, lp == 15, [wn, bn], [pn])
                dst = hidk[:, hc, :] if kv == 0 else hidv[:, hc, pc0:pc0 + 32]
                S.op('act', lambda e: e.activation(out=dst, in_=p[:, 0:32], func=AF.Gelu_apprx_tanh, bias=b1[:, c4:c4 + 1]),
                     reads=[pn, 'b1'], writes=['hidk' if kv == 0 else 'hidv'])
            p = pA[nxt('pA')]; pn = f"pA{rot['pA'] ^ 1}"
            for hc in range(2):
                mm(p[0:64, 0:32], w2s[:, 0, hc, :], hidk[:, hc, :], hc == 0, hc == 1, ['w2s', 'hidk'], [pn])
            S.op('act', lambda e: e.activation(out=kcT[:, sl0:sl0 + 32], in_=p[0:64, 0:32], func=AF.Copy), reads=[pn], writes=['kcT'])
            p = pA[nxt('pA')]; pn = f"pA{rot['pA'] ^ 1}"
            for hc in range(2):
                mm(p[:, 0:64], hidv[:, hc, :], w2s[:, 1, hc, :], hc == 0, hc == 1, ['w2s', 'hidv'], [pn])
            S.op('dve', lambda e: e.tensor_tensor(out=vc[:, jb_new, 0:64], in0=p[:, 0:64], in1=vc[:, jb_new, 0:64], op=ALU.add), reads=[pn, 'vc'], writes=['vc'])

            yi = nxt('yT'); yt = yT[yi]; yn = f"yT{yi}"
            S.op('dve', lambda e: e.tensor_scalar(out=yt[:], in0=uT[:], scalar1=dcol[:, 0:1], scalar2=None, op0=ALU.mult), reads=['uT', 'dcol'], writes=[yn])

            def ssm_unit(sc, j):
                c0 = sc * 256
                pre = pA[nxt('pA')]; pren = f"pA{rot['pA'] ^ 1}"
                mm(pre[:, 0:256], BBre[:, j * 128:(j + 1) * 128], uT[:, c0:c0 + 256], True, True, ['BB', 'uT'], [pren])
                mm(pre[:, 256:512], BBim[:, j * 128:(j + 1) * 128], uT[:, c0:c0 + 256], True, True, ['BB', 'uT'], [pren], skip=True)
                cs_, sn_ = cosT[:, j, :], sinT[:, j, :]
                bre, bim = pre[:, 0:256], pre[:, 256:512]
                S.op('dve', lambda e: e.tensor_tensor(out=sw[0][:], in0=bre, in1=cs_, op=ALU.mult), reads=[pren] + TRIG, writes=['sw0'])
                S.op('dve', lambda e: e.tensor_tensor(out=sw[1][:], in0=bim, in1=sn_, op=ALU.mult), reads=[pren] + TRIG, writes=['sw1'])
                S.op('pool', lambda e: e.tensor_tensor(out=sw[0][:], in0=sw[0][:], in1=sw[1][:], op=ALU.add), reads=['sw0', 'sw1'], writes=['sw0'])
                S.op('dve', lambda e: e.tensor_tensor(out=sw[2][:], in0=bim, in1=cs_, op=ALU.mult), reads=[pren] + TRIG, writes=['sw2'])
                S.op('dve', lambda e: e.tensor_tensor(out=sw[3][:], in0=bre, in1=sn_, op=ALU.mult), reads=[pren] + TRIG, writes=['sw3'])
                S.op('pool', lambda e: e.tensor_tensor(out=sw[2][:], in0=sw[2][:], in1=sw[3][:], op=ALU.subtract), reads=['sw2', 'sw3'], writes=['sw2'])
                S.op('pool', lambda e: e.tensor_tensor(out=ctmp[:, 0:1], in0=carry_re[:, j:j + 1], in1=erc[:, j:j + 1], op=ALU.mult), reads=['carry'] + TRIG, writes=['ctmp'])
                S.op('pool', lambda e: e.tensor_tensor(out=ctmp[:, 1:2], in0=carry_im[:, j:j + 1], in1=eic[:, j:j + 1], op=ALU.mult), reads=['carry'] + TRIG, writes=['ctmp'])
                S.op('pool', lambda e: e.tensor_tensor(out=cin_re[:], in0=ctmp[:, 0:1], in1=ctmp[:, 1:2], op=ALU.subtract), reads=['ctmp'], writes=['cin'])
                S.op('pool', lambda e: e.tensor_tensor(out=ctmp[:, 0:1], in0=carry_re[:, j:j + 1], in1=eic[:, j:j + 1], op=ALU.mult), reads=['carry'] + TRIG, writes=['ctmp'])
                S.op('pool', lambda e: e.tensor_tensor(out=ctmp[:, 1:2], in0=carry_im[:, j:j + 1], in1=erc[:, j:j + 1], op=ALU.mult), reads=['carry'] + TRIG, writes=['ctmp'])
                S.op('pool', lambda e: e.tensor_tensor(out=cin_im[:], in0=ctmp[:, 0:1], in1=ctmp[:, 1:2], op=ALU.add), reads=['ctmp'], writes=['cin'])
                S.op('dve', lambda e: e.tensor_tensor_scan(out=sw[4][:], data0=rcolS[:, j:j + 1].to_broadcast([128, 256]), data1=sw[0][:],
                                                           initial=cin_re[:], op0=ALU.mult, op1=ALU.add), reads=['sw0', 'cin', 'rcolS'], writes=['sw4'])
                S.op('dve', lambda e: e.tensor_tensor_scan(out=sw[5][:], data0=rcolS[:, j:j + 1].to_broadcast([128, 256]), data1=sw[2][:],
                                                           initial=cin_im[:], op0=ALU.mult, op1=ALU.add), reads=['sw2', 'cin', 'rcolS'], writes=['sw5'])
                S.op('pool', lambda e: e.tensor_tensor(out=sw[6][:], in0=sw[4][:], in1=cs_, op=ALU.mult), reads=['sw4'] + TRIG, writes=['sw6'])
                S.op('pool', lambda e: e.tensor_tensor(out=sw[7][:], in0=sw[5][:], in1=sn_, op=ALU.mult), reads=['sw5'] + TRIG, writes=['sw7'])
                S.op('pool', lambda e: e.tensor_tensor(out=hre[:, j, c0:c0 + 256], in0=sw[6][:], in1=sw[7][:], op=ALU.subtract), reads=['sw6', 'sw7'], writes=['hre'])
                S.op('pool', lambda e: e.tensor_tensor(out=sw[6][:], in0=sw[4][:], in1=sn_, op=ALU.mult), reads=['sw4'] + TRIG, writes=['sw6'])
                S.op('pool', lambda e: e.tensor_tensor(out=sw[7][:], in0=sw[5][:], in1=cs_, op=ALU.mult), reads=['sw5'] + TRIG, writes=['sw7'])
                S.op('pool', lambda e: e.tensor_tensor(out=him[:, j, c0:c0 + 256], in0=sw[6][:], in1=sw[7][:], op=ALU.add), reads=['sw6', 'sw7'], writes=['him'])
                S.op('act', lambda e: e.activation(out=carry_re[:, j:j + 1], in_=hre[:, j, c0 + 255:c0 + 256], func=AF.Copy), reads=['hre'], writes=['carry'])
                S.op('act', lambda e: e.activation(out=carry_im[:, j:j + 1], in_=him[:, j, c0 + 255:c0 + 256], func=AF.Copy), reads=['him'], writes=['carry'])
            def ssm_finish(yt=yt, yn=yn, t0=t0):
                for sc in range(2):
                    c0 = sc * 256
                    py = pA[nxt('pA')]; pyn = f"pA{rot['pA'] ^ 1}"
                    for j in range(4):
                        mm(py[:, 0:256], CCre[:, j, :], hre[:, j, c0:c0 + 256], j == 0, False, ['CC', 'hre'], [pyn])
                        mm(py[:, 0:256], CCim[:, j, :], him[:, j, c0:c0 + 256], False, j == 3, ['CC', 'him'], [pyn])
                    S.op('dve', lambda e: e.tensor_tensor(out=yt[:, c0:c0 + 256], in0=py[:, 0:256], in1=yt[:, c0:c0 + 256], op=ALU.add), reads=[pyn, yn], writes=[yn])

                S.op('act', lambda e: e.activation(out=yt[:], in_=yt[:], func=AF.Gelu_apprx_tanh), reads=[yn], writes=[yn])
                S.dma_start('sp', o_ssm[:, t0:t0 + 512], yt[:], reads=[yn], is_output=True)


            for ts in range(4):
                i = I * 4 + ts
                Qi = qT[:, ts, :, :].rearrange("p a b -> p (a b)")

                def branch(br, blocks):
                    oi = nxt('pO'); po = pO[oi]; pon = f"pO{oi}"
                    S.op('act', lambda e: e.activation(out=po[:], in_=zer[:, 0:260].rearrange("p (a b) -> p a b", a=4), func=AF.Copy), reads=['zer'], writes=[pon])
                    if br == 0:
                        S.op('act', lambda e: e.activation(out=pI[:], in_=zer[:, :].rearrange("p (a b) -> p a b", a=4), func=AF.Copy), reads=['zer'], writes=['pI'])

                    def stage1(blk):
                        (kl, kreads, masks, vr, vreads, ovr) = blk
                        masks = [m_() if callable(m_) else m_ for m_ in masks]
                        si = nxt('pS', 3); psn = f"pS{si}"; p = pS[si]
                        mm(p[:], kl, Qi, True, len(masks) == 0, kreads + ['qT'], [psn])
                        for mi, (ml, mreads) in enumerate(masks):
                            mm(p[:], ml, id4[:], False, mi == len(masks) - 1, mreads + ['id4'], [psn])
                        return p, psn

                    def stage2(blk, p, psn):
                        (kl, kreads, masks, vr, vreads, ovr) = blk
                        pi_ = nxt('pT'); ptile = pT[pi_]; ptn = f"pT{pi_}"
                        S.op('act', lambda e: e.activation(out=ptile[:], in_=p[:], func=AF.Exp, scale=0.125), reads=[psn], writes=[ptn])
                        for h in range(4):
                            mm(po[:, h, :], ptile[:, h * 128:(h + 1) * 128], vr, False, False, [ptn] + vreads, [pon], skip=True)
                            if ovr is not None:
                                mm(pI[:, h, :], ptile[:, h * 128:(h + 1) * 128], ovr, False, False, [ptn, 'ov'], ['pI'], skip=True)

                    prev = None
                    for blk in blocks:
                        cur = stage1(blk)
                        if prev is not None:
                            stage2(*prev)
                        prev = (blk,) + cur
                    stage2(*prev)
                    S.op('act', lambda e: e.activation(out=osb[br][:], in_=po[:], func=AF.Copy), reads=[pon], writes=[f'osb{br}'])
                    S.op('dve', lambda e: e.tensor_scalar(out=den[:, br, :], in0=osb[br][:, :, 64], scalar1=1e-30, scalar2=None, op0=ALU.max),
                         reads=[f'osb{br}'], writes=['den'])
                    S.op('dve', lambda e: e.reciprocal(out=den[:, br, :], in_=den[:, br, :]), reads=['den'], writes=['den'])

                nb = (8 * i + 7) // 128 + 1
                blocks = []
                for jb in range(nb):
                    masks = []
                    if (i, jb) in _CMKEY:
                        def mc_(jb=jb):
                            ci = nxt('cmt'); ct = cmt[ci]; cn = f"cmt{ci}"
                            S.dma_start('sp', ct[:], c_cm[_CMKEY[(i, jb)]], writes=[cn])
                            return (ct[:], [cn])
                        masks.append(mc_)
                    blocks.append((kcT[:, jb * 128:(jb + 1) * 128], ['kcT'], masks, vc[:, jb, :], ['vc'], ov[:, jb, :]))
                branch(0, blocks)
                S.op('dve', lambda e: e.tensor_scalar(out=imp[:], in0=pI[:, 0, :], scalar1=den[:, 0, 0:1], scalar2=None, op0=ALU.mult), reads=['pI', 'den'], writes=['imp'])
                for h in range(1, 4):
                    S.op('dve', lambda e: e.scalar_tensor_tensor(out=imp[:], in0=pI[:, h, :], scalar=den[:, 0, h:h + 1], in1=imp[:], op0=ALU.mult, op1=ALU.add),
                         reads=['pI', 'den', 'imp'], writes=['imp'])
                off = 128 - 2 * i
                S.op('dve', lambda e: e.tensor_tensor(out=imp2[:], in0=imp[:], in1=keep[:, off:off + 128], op=ALU.mult), reads=['imp', 'keep'], writes=['imp2'])
                S.op('dve', lambda e: e.tensor_tensor(out=imp2[:], in0=imp2[:], in1=addt[:, off:off + 128], op=ALU.add), reads=['imp2', 'add'], writes=['imp2'])
                S.op('dve', lambda e: e.memset(imp2[:, 0:1], 3.0e4), writes=['imp2'])
                S.op('dve', lambda e: e.max(out=m8[:], in_=imp2[:]), reads=['imp2'], writes=['m8'])
                S.op('dve', lambda e: e.match_replace(out=wk[:], in_to_replace=m8[:], in_values=imp2[:], imm_value=-5.0), reads=['imp2', 'm8'], writes=['wk'])
                S.op('dve', lambda e: e.max(out=m8b[:], in_=wk[:]), reads=['wk'], writes=['m8b'])
                S.op('dve', lambda e: e.tensor_scalar(out=negm[:], in0=imp2[:], scalar1=m8b[:, 7:8], scalar2=NEGM, op0=ALU.is_lt, op1=ALU.mult),
                     reads=['imp2', 'm8b'], writes=['negm'])
                if debug:
                    S.dma_start('sp', o_dbg[i, :, 0, :], imp[:], reads=['imp'], is_output=True)
                    S.dma_start('sp', o_dbg[i, :, 1, :], imp2[:], reads=['imp2'], is_output=True)
                    S.dma_start('sp', o_dbg[i, :, 2, :], negm[:], reads=['negm'], is_output=True)
                    S.dma_start('sp', o_dbg[i, :, 3, :], wk[:], reads=['wk'], is_output=True)
                blocks = []
                for j in range(max(0, i - 4), i + 1):
                    masks = []
                    if j == i:
                        masks.append((triq[:], ['triq']))
                    if j == i - 4:
                        masks.append((tri2q[:], ['tri2q']))
                    s8 = j % 8
                    blocks.append((kwr[:, s8 * 128:(s8 + 1) * 128], [f'kwr{s8 // 4}'], masks, vwr[:, s8, :], [f'vwr{s8}'], None))
                branch(2, blocks)
                blocks = []
                for j in range(i + 1):
                    def mk_(j=j):
                        ni = nxt('ne'); ne = negexp[ni]; nn = f"negexp{ni}"
                        S.op('act', lambda e: e.activation(out=ne[:].rearrange("p (a b) -> p a b", a=2), in_=negm[:, 2 * j:2 * j + 2].unsqueeze(2).to_broadcast([128, 2, 64]), func=AF.Copy),
                             reads=['negm'], writes=[nn])
                        return (ne[:], [nn])
                    masks = [mk_]
                    if j == i:
                        masks.append((triq[:], ['triq']))
                    blocks.append((KT[:, j * 128:(j + 1) * 128], [f'KT{j // 4}'], masks, vslc[:, j, :], [f'vslc{j}'], None))
                branch(1, blocks)
                S.op('dve', lambda e: e.tensor_tensor(out=coef[:].rearrange("p a b -> p (a b)"), in0=den[:].rearrange("p a b -> p (a b)"), in1=gts[:, ts, :], op=ALU.mult),
                     reads=['den', 'gts'], writes=['coef'])
                ai = nxt('aout'); ao = aout[ai]; an_ = f"aout{ai}"
                for h in range(4):
                    S.op('dve', lambda e: e.tensor_scalar(out=ao[:, h * 64:(h + 1) * 64], in0=osb[0][:, h, 0:64], scalar1=coef[:, 0, h:h + 1], scalar2=None, op0=ALU.mult),
                         reads=['osb0', 'coef'], writes=[an_])
                    for br in (1, 2):
                        S.op('dve', lambda e: e.scalar_tensor_tensor(out=ao[:, h * 64:(h + 1) * 64], in0=osb[br][:, h, 0:64], scalar=coef[:, br, h:h + 1],
                                                                     in1=ao[:, h * 64:(h + 1) * 64], op0=ALU.mult, op1=ALU.add),
                             reads=[f'osb{br}', 'coef', an_], writes=[an_])
                S.dma_start('sp', o_attn[i * 128:(i + 1) * 128, :], ao[:], reads=[an_], is_output=True)
                if ts == 0 and pending_fin:
                    pending_fin.pop()()
                for u_ in (2 * ts, 2 * ts + 1):
                    ssm_unit(u_ // 4, u_ % 4)
            pending_fin.append(ssm_finish)
        pending_fin.pop()()
        S.finish('sp')
    return nc, S


def _barrier(S):
    toks = []
    for k in S.eng:
        if S.cnt[k] > 0:
            toks.append((S.sem[k][0], S.sem[k][1], S.cnt[k], k))
    for h, val, sid in S.dma:
        if val > 0:
            toks.append((h, sid, val, 'dma'))
    for e in S.eng:
        for t in toks:
            S._wait(e, t)


NTOK = 2048


def build_B():
    nc = bass.Bass("TRN2", target_bir_lowering=False)

    def din(name, shape):
        return nc.dram_tensor(name, list(shape), F32, kind="ExternalInput").ap()

    xT = din("xT", [8, 128, NTOK]); aT = din("aT", [8, 128, NTOK]); sT = din("sT", [4, 128, NTOK])
    cst = din("cst", [128, 8])
    adaw = din("adaw", [48, 128, 8, 128]); adab = din("adab", [128, 48])
    n1g = din("n1g", [128, 8]); n2g = din("n2g", [128, 8]); fing = din("fing", [128, 8])
    wmg = din("wmg", [16, 128, 8, 128])
    wglu = din("wglu", [4, 128, 4, 128])
    wnso = din("wnso", [8, 128, 8, 128])
    wsso = din("wsso", [8, 128, 4, 128])
    wout = din("wout", [8, 128, 8, 128])
    wr = din("wr", [128, 8, 36]); br = din("br", [1, 36])
    mwg = din("mwg", [32, 1024, 256]); mwu = din("mwu", [32, 1024, 256]); mwd = din("mwd", [32, 256, 1024])
    c_ident = din("ident", [128, 128]); c_R = din("Rsel", [32, 4096])
    o_x = nc.dram_tensor("o_x", [8, 128, NTOK], F32, kind="ExternalOutput").ap()
    o_n = nc.dram_tensor("o_n", [8, 128, NTOK], F32, kind="ExternalOutput").ap()

    S = Sch(nc)
    with contextlib.ExitStack() as st:
        def sb(name, shape, dt=F32):
            return st.enter_context(nc.sbuf_tensor(name, list(shape), dt))

        def ps(name, shape):
            return st.enter_context(nc.psum_tensor(name, list(shape), F32))

        ones = sb("ones", [128, 128]); ident = sb("ident_s", [128, 128]); Rsel = sb("Rsel_s", [32, 4096])
        modc = sb("modc", [128, 48]); scl1 = sb("scl1", [128, 8]); scl2 = sb("scl2", [128, 8]); fg = sb("fg", [128, 8])
        biasmg = sb("biasmg", [1, 2048]); wrs = sb("wrs", [128, 8, 36]); brs = sb("brs", [1, 36])
        xt = sb("xt", [128, 8, 512]); x1 = sb("x1", [128, 8, 512]); h2 = sb("h2", [128, 8, 512])
        xsq = [sb(f"xsq{i}", [128, 512]) for i in range(2)]
        sqbc = sb("sqbc", [128, 512]); rbc = sb("rbc", [128, 512])
        wgtT = sb("wgtT", [32, 512])
        lg = sb("lg", [128, 36]); gmax = sb("gmax", [128, 1]); ngmax = sb("ngmax", [128, 1]); eg = sb("eg", [128, 4]); gsum = sb("gsum", [128, 1])
        oh = sb("oh", [128, 4]); msk = sb("msk", [128, 32]); m8 = sb("m8", [128, 8]); nl1 = sb("nl1", [128, 1]); ew = sb("ew", [128, 32])
        sel2 = sb("sel2", [128, 32]); s2 = sb("s2", [128, 1]); wgt = sb("wgt", [128, 32])

        pA = [ps(f"pA{i}", [128, 512]) for i in range(2)]
        pG = [ps(f"pG{i}", [128, 512]) for i in range(2)]
        pU = [ps(f"pU{i}", [128, 512]) for i in range(2)]
        pD = [ps(f"pD{i}", [128, 512]) for i in range(2)]
        rot = {}

        def nxt(k, n=2):
            v = rot.get(k, 0); rot[k] = (v + 1) % n
            return v

        def mm(out, lhsT, rhs, start, stop, reads, writes):
            S.op('pe', lambda e: e.matmul(out, lhsT=lhsT, rhs=rhs, start=start, stop=stop), reads=reads, writes=writes)

        def pget(pool, name):
            i = nxt(name)
            return pool[i], f"{name}{i}"

        S.dma_start('sp', ident[:], c_ident, writes=['ident'])
        S.dma_start('sp', Rsel[:], c_R, writes=['Rsel'])
        S.dma_start('sp', wrs[:], wr, writes=['wrs'])
        S.dma_start('sp', brs[:], br, writes=['brs'])
        S.dma_start('sp', fg[:], fing, writes=['fg'])
        S.op('pool', lambda e: e.memset(ones[:], 1.0), writes=['ones'])

        with contextlib.ExitStack() as st2:
            def sb2(name, shape, dt=F32):
                return st2.enter_context(nc.sbuf_tensor(name, list(shape), dt))
            cs = sb2("cs", [128, 8]); sg = sb2("sg", [128, 8])
            slab = [sb2(f"pslab{i}", [128, 8, 128]) for i in range(2)]
            adabs = sb2("adabs", [128, 48]); n1 = sb2("n1", [128, 8]); n2 = sb2("n2", [128, 8])
            S.dma_start('sp', cs[:], cst, writes=['cs'])
            S.dma_start('sp', adabs[:], adab, writes=['adabs'])
            S.dma_start('sp', n1[:], n1g, writes=['n1']); S.dma_start('sp', n2[:], n2g, writes=['n2'])
            S.op('act', lambda e: e.activation(out=sg[:], in_=cs[:], func=AF.Sigmoid), reads=['cs'], writes=['sg'])
            S.op('dve', lambda e: e.tensor_tensor(out=cs[:], in0=cs[:], in1=sg[:], op=ALU.mult), reads=['cs', 'sg'], writes=['cs'])
            for j in range(48):
                a = slab[j % 2]; an = f"pslab{j % 2}"
                S.dma_start('sp', a[:], adaw[j], writes=[an])
                p, pn = pget(pA, 'pA')
                for kc in range(8):
                    mm(p[:, 0:1], a[:, kc, :], cs[:, kc:kc + 1], kc == 0, kc == 7, [an, 'cs'], [pn])
                S.op('dve', lambda e: e.tensor_tensor(out=modc[:, j:j + 1], in0=p[:, 0:1], in1=adabs[:, j:j + 1], op=ALU.add),
                     reads=[pn, 'adabs'], writes=['modc'])
            for c in range(16):
                a = slab[c % 2]; an = f"pslab{c % 2}"
                S.dma_start('sp', a[:], wmg[c], writes=[an])
                p, pn = pget(pA, 'pA')
                for kc in range(8):
                    mm(p[0:1, 0:128], modc[:, kc:kc + 1], a[:, kc, :], kc == 0, kc == 7, ['modc', an], [pn])
                S.op('act', lambda e: e.activation(out=biasmg[0:1, c * 128:(c + 1) * 128], in_=p[0:1, 0:128], func=AF.Copy), reads=[pn], writes=['biasmg'])
            S.op('dve', lambda e: e.tensor_scalar(out=scl1[:], in0=modc[:, 8:16], scalar1=1.0, scalar2=None, op0=ALU.add), reads=['modc'], writes=['scl1'])
            S.op('dve', lambda e: e.tensor_tensor(out=scl1[:], in0=scl1[:], in1=n1[:], op=ALU.mult), reads=['scl1', 'n1'], writes=['scl1'])
            S.op('dve', lambda e: e.tensor_scalar(out=scl2[:], in0=modc[:, 32:40], scalar1=1.0, scalar2=None, op0=ALU.add), reads=['modc'], writes=['scl2'])
            S.op('dve', lambda e: e.tensor_tensor(out=scl2[:], in0=scl2[:], in1=n2[:], op=ALU.mult), reads=['scl2', 'n2'], writes=['scl2'])
        _barrier(S)

        def rms_stats(src, srcname):
            p, pn = pget(pA, 'pA')
            for kc in range(8):
                xi = nxt('xsq'); xs = xsq[xi]; xn = f"xsq{xi}"
                S.op('act', lambda e: e.activation(out=xs[:], in_=src[:, kc, :], func=AF.Square), reads=[srcname], writes=[xn])
                mm(p[:], ones[:], xs[:], kc == 0, kc == 7, ['ones', xn], [pn])
            S.op('act', lambda e: e.activation(out=sqbc[:], in_=p[:], func=AF.Sqrt, scale=1.0 / D, bias=EPS), reads=[pn], writes=['sqbc'])
            S.op('dve', lambda e: e.reciprocal(out=rbc[:], in_=sqbc[:]), reads=['sqbc'], writes=['rbc'])

        for T in range(NTOK // 512):
            t0 = T * 512
            with contextlib.ExitStack() as st3:
                def sb3(name, shape, dt=F32):
                    return st3.enter_context(nc.sbuf_tensor(f"{name}_t{T}", list(shape), dt))
                at = sb3("at", [128, 8, 512]); stt = sb3("stt", [128, 4, 512]); hs = sb3("hs", [128, 8, 512])
                ssmg = sb3("ssmg", [128, 4, 512]); merged = sb3("merged", [128, 8, 512])
                slabs = [sb3(f"slab{i}", [128, 8, 128]) for i in range(4)]
                ga = sb3("ga", [128, 512]); gs = sb3("gs", [128, 512]); m2 = sb3("m2", [128, 512]); tmp = sb3("tmp", [128, 512])

                def get_slab(src, kcn):
                    i = nxt('slab', 4)
                    S.dma_start('sp', slabs[i][:, 0:kcn, :], src, writes=[f"slab{i}"])
                    return slabs[i], f"slab{i}"

                S.dma_start('sp', xt[:], xT[:, :, t0:t0 + 512].rearrange("k p t -> p k t"), writes=['xt'])
                S.dma_start('sp', at[:], aT[:, :, t0:t0 + 512].rearrange("k p t -> p k t"), writes=['at'])
                S.dma_start('sp', stt[:], sT[:, :, t0:t0 + 512].rearrange("k p t -> p k t"), writes=['stt'])
                rms_stats(xt, 'xt')
                for kc in range(8):
                    S.op('pool', lambda e: e.tensor_scalar(out=hs[:, kc, :], in0=xt[:, kc, :], scalar1=scl1[:, kc:kc + 1], scalar2=None, op0=ALU.mult),
                         reads=['xt', 'scl1'], writes=['hs'])
                for c in range(4):
                    sl, sn = get_slab(wglu[c], 4)
                    p, pn = pget(pA, 'pA')
                    for kc in range(4):
                        mm(p[:], sl[:, kc, :], stt[:, kc, :], kc == 0, kc == 3, [sn, 'stt'], [pn])
                    S.op('act', lambda e: e.activation(out=tmp[:], in_=p[:], func=AF.Sigmoid), reads=[pn], writes=['tmp'])
                    S.op('dve', lambda e: e.tensor_tensor(out=ssmg[:, c, :], in0=stt[:, c, :], in1=tmp[:], op=ALU.mult), reads=['stt', 'tmp'], writes=['ssmg'])
                for c in range(8):
                    for (gi, gdst, gname) in ((c, ga, 'ga'), (8 + c, gs, 'gs')):
                        sl, sn = get_slab(wmg[gi], 8)
                        p, pn = pget(pA, 'pA')
                        for kc in range(8):
                            mm(p[:], sl[:, kc, :], hs[:, kc, :], kc == 0, False, [sn, 'hs'], [pn])
                        mm(p[:], biasmg[0:1, gi * 128:(gi + 1) * 128], sqbc[0:1, :], False, True, ['biasmg', 'sqbc'], [pn])
                        S.op('dve', lambda e: e.tensor_tensor(out=gdst[:], in0=p[:], in1=rbc[:], op=ALU.mult), reads=[pn, 'rbc'], writes=[gname])
                        S.op('act', lambda e: e.activation(out=gdst[:], in_=gdst[:], func=AF.Sigmoid), reads=[gname], writes=[gname])
                    sl, sn = get_slab(wnso[c], 8)
                    p, pn = pget(pA, 'pA')
                    for kc in range(8):
                        mm(p[:], sl[:, kc, :], at[:, kc, :], kc == 0, kc == 7, [sn, 'at'], [pn])
                    S.op('dve', lambda e: e.tensor_tensor(out=merged[:, c, :], in0=p[:], in1=ga[:], op=ALU.mult), reads=[pn, 'ga'], writes=['merged'])
                    sl, sn = get_slab(wsso[c], 4)
                    p, pn = pget(pA, 'pA')
                    for kc in range(4):
                        mm(p[:], sl[:, kc, :], ssmg[:, kc, :], kc == 0, kc == 3, [sn, 'ssmg'], [pn])
                    S.op('dve', lambda e: e.tensor_tensor(out=m2[:], in0=p[:], in1=gs[:], op=ALU.mult), reads=[pn, 'gs'], writes=['m2'])
                    S.op('pool', lambda e: e.tensor_tensor(out=merged[:, c, :], in0=merged[:, c, :], in1=m2[:], op=ALU.add), reads=['merged', 'm2'], writes=['merged'])
                for c in range(8):
                    sl, sn = get_slab(wout[c], 8)
                    p, pn = pget(pA, 'pA')
                    for kc in range(8):
                        mm(p[:], sl[:, kc, :], merged[:, kc, :], kc == 0, kc == 7, [sn, 'merged'], [pn])
                    S.op('dve', lambda e: e.scalar_tensor_tensor(out=x1[:, c, :], in0=p[:], scalar=modc[:, 16 + c:17 + c], in1=xt[:, c, :], op0=ALU.mult, op1=ALU.add),
                         reads=[pn, 'modc', 'xt'], writes=['x1'])
                rms_stats(x1, 'x1')
                for kc in range(8):
                    S.op('dve', lambda e: e.tensor_tensor(out=h2[:, kc, :], in0=x1[:, kc, :], in1=rbc[:], op=ALU.mult), reads=['x1', 'rbc'], writes=['h2'])
                    S.op('pool', lambda e: e.tensor_scalar(out=h2[:, kc, :], in0=h2[:, kc, :], scalar1=scl2[:, kc:kc + 1], scalar2=modc[:, 24 + kc:25 + kc], op0=ALU.mult, op1=ALU.add),
                         reads=['h2', 'scl2', 'modc'], writes=['h2'])
                for ts in range(4):
                    p, pn = pget(pA, 'pA')
                    for kc in range(8):
                        mm(p[:, 0:36], h2[:, kc, ts * 128:(ts + 1) * 128], wrs[:, kc, :], kc == 0, False, ['h2', 'wrs'], [pn])
                    mm(p[:, 0:36], ones[0:1, 0:128], brs[0:1, :], False, True, ['ones', 'brs'], [pn])
                    S.op('act', lambda e: e.activation(out=lg[:], in_=p[:, 0:36], func=AF.Copy), reads=[pn], writes=['lg'])
                    dv = lambda fn, r, w: S.op('dve', fn, reads=r, writes=w)
                    dv(lambda e: e.tensor_reduce(out=gmax[:], in_=lg[:, 0:4], axis=AX.X, op=ALU.max), ['lg'], ['gmax'])
                    dv(lambda e: e.tensor_scalar(out=ngmax[:], in0=gmax[:], scalar1=-1.0, scalar2=None, op0=ALU.mult), ['gmax'], ['ngmax'])
                    S.op('act', lambda e: e.activation(out=eg[:], in_=lg[:, 0:4], func=AF.Exp, bias=ngmax[:, 0:1]), reads=['lg', 'ngmax'], writes=['eg'])
                    dv(lambda e: e.tensor_reduce(out=gsum[:], in_=eg[:], axis=AX.X, op=ALU.add), ['eg'], ['gsum'])
                    dv(lambda e: e.reciprocal(out=gsum[:], in_=gsum[:]), ['gsum'], ['gsum'])
                    dv(lambda e: e.tensor_scalar(out=oh[:], in0=lg[:, 0:4], scalar1=gmax[:, 0:1], scalar2=None, op0=ALU.is_ge), ['lg', 'gmax'], ['oh'])
                    dv(lambda e: e.tensor_scalar(out=oh[:], in0=oh[:], scalar1=1.0, scalar2=1.0e9, op0=ALU.subtract, op1=ALU.mult), ['oh'], ['oh'])
                    for g in range(4):
                        dv(lambda e: e.tensor_scalar(out=msk[:, g * 8:(g + 1) * 8], in0=lg[:, 4 + g * 8:12 + g * 8], scalar1=oh[:, g:g + 1], scalar2=None, op0=ALU.add),
                           ['lg', 'oh'], ['msk'])
                    dv(lambda e: e.max(out=m8[:], in_=msk[:]), ['msk'], ['m8'])
                    dv(lambda e: e.tensor_scalar(out=nl1[:], in0=m8[:, 0:1], scalar1=-1.0, scalar2=None, op0=ALU.mult), ['m8'], ['nl1'])
                    S.op('act', lambda e: e.activation(out=ew[:], in_=msk[:], func=AF.Exp, bias=nl1[:, 0:1]), reads=['msk', 'nl1'], writes=['ew'])
                    dv(lambda e: e.tensor_scalar(out=sel2[:], in0=msk[:], scalar1=m8[:, 1:2], scalar2=None, op0=ALU.is_ge), ['msk', 'm8'], ['sel2'])
                    dv(lambda e: e.tensor_tensor(out=ew[:], in0=ew[:], in1=sel2[:], op=ALU.mult), ['ew', 'sel2'], ['ew'])
                    dv(lambda e: e.tensor_reduce(out=s2[:], in_=ew[:], axis=AX.X, op=ALU.add), ['ew'], ['s2'])
                    dv(lambda e: e.reciprocal(out=s2[:], in_=s2[:]), ['s2'], ['s2'])
                    dv(lambda e: e.tensor_tensor(out=s2[:], in0=s2[:], in1=gsum[:], op=ALU.mult), ['s2', 'gsum'], ['s2'])
                    dv(lambda e: e.tensor_scalar(out=wgt[:], in0=ew[:], scalar1=s2[:, 0:1], scalar2=None, op0=ALU.mult), ['ew', 's2'], ['wgt'])
                    p2, pn2 = pget(pA, 'pA')
                    S.op('pe', lambda e: e.transpose(out=p2[0:32, 0:128], in_=wgt[:], identity=ident[:]), reads=['wgt', 'ident'], writes=[pn2])
                    S.op('act', lambda e: e.activation(out=wgtT[:, ts * 128:(ts + 1) * 128], in_=p2[0:32, 0:128], func=AF.Copy), reads=[pn2], writes=['wgtT'])
            _barrier(S)
            with contextlib.ExitStack() as st4:
                def sb4(name, shape, dt=F32):
                    return st4.enter_context(nc.sbuf_tensor(f"{name}_t{T}", list(shape), dt))
                acc = sb4("acc", [128, 8, 512])
                wg = [sb4(f"wg{i}", [128, 8, 256]) for i in range(2)]
                wu = [sb4(f"wu{i}", [128, 8, 256]) for i in range(2)]
                wd = [sb4(f"wd{i}", [128, 2, 1024]) for i in range(2)]
                hid = [sb4(f"hid{i}", [128, 512]) for i in range(4)]
                actb = [sb4(f"actb{i}", [128, 512]) for i in range(2)]
                wbc = [sb4(f"wbc{i}", [128, 512]) for i in range(2)]
                S.op('pool', lambda e: e.memset(acc[:], 0.0), writes=[f'acc{c}' for c in range(8)])
                def down(ex):
                    b = ex % 2
                    for c in range(8):
                        pd, pdn = pget(pD, 'pD')
                        for hc in range(2):
                            mm(pd[:], wd[b][:, hc, c * 128:(c + 1) * 128], hid[b * 2 + hc][:], hc == 0, hc == 1, [f'wd{b}', f'hid{b * 2 + hc}'], [pdn])
                        S.op('dve', lambda e: e.tensor_tensor(out=acc[:, c, :], in0=pd[:], in1=acc[:, c, :], op=ALU.add), reads=[pdn, f'acc{c}'], writes=[f'acc{c}'])

                for ex in range(32):
                    b = ex % 2
                    S.dma_start('sp', wg[b][:], mwg[ex].rearrange("(k p) n -> p k n", p=128), writes=[f'wg{b}'])
                    S.dma_start('sp', wu[b][:], mwu[ex].rearrange("(k p) n -> p k n", p=128), writes=[f'wu{b}'])
                    S.dma_start('sp', wd[b][:], mwd[ex].rearrange("(k p) n -> p k n", p=128), writes=[f'wd{b}'])
                    p, pn = pget(pA, 'pA')
                    mm(p[:], Rsel[:, ex * 128:(ex + 1) * 128], wgtT[:], True, True, ['Rsel', 'wgtT'], [pn])
                    S.op('act', lambda e: e.activation(out=wbc[b][:], in_=p[:], func=AF.Copy), reads=[pn], writes=[f'wbc{b}'])
                    for hc in range(2):
                        hi = b * 2 + hc
                        pg, pgn = pget(pG, 'pG')
                        pu, pun = pget(pU, 'pU')
                        for kc in range(8):
                            mm(pg[:], wg[b][:, kc, hc * 128:(hc + 1) * 128], h2[:, kc, :], kc == 0, kc == 7, [f'wg{b}', 'h2'], [pgn])
                        for kc in range(8):
                            mm(pu[:], wu[b][:, kc, hc * 128:(hc + 1) * 128], h2[:, kc, :], kc == 0, kc == 7, [f'wu{b}', 'h2'], [pun])
                        S.op('act', lambda e: e.activation(out=actb[hc][:], in_=pg[:], func=AF.Silu), reads=[pgn], writes=[f'actb{hc}'])
                        S.op('dve', lambda e: e.tensor_tensor(out=hid[hi][:], in0=pu[:], in1=actb[hc][:], op=ALU.mult), reads=[pun, f'actb{hc}'], writes=[f'hid{hi}'])
                        S.op('pool', lambda e: e.tensor_tensor(out=hid[hi][:], in0=hid[hi][:], in1=wbc[b][:], op=ALU.mult), reads=[f'hid{hi}', f'wbc{b}'], writes=[f'hid{hi}'])
                    if ex > 0:
                        down(ex - 1)
                down(31)
                for c in range(8):
                    S.op('dve', lambda e: e.scalar_tensor_tensor(out=acc[:, c, :], in0=acc[:, c, :], scalar=modc[:, 40 + c:41 + c], in1=x1[:, c, :], op0=ALU.mult, op1=ALU.add),
                         reads=[f'acc{c}', 'modc', 'x1'], writes=[f'acc{c}', 'accall'])
                S.dma_start('sp', o_x[:, :, t0:t0 + 512].rearrange("k p t -> p k t"), acc[:], reads=['accall'] + [f'acc{c}' for c in range(8)], is_output=True)
                rms_stats(acc, 'accall')
                for c in range(8):
                    S.op('dve', lambda e: e.tensor_tensor(out=h2[:, c, :], in0=acc[:, c, :], in1=rbc[:], op=ALU.mult), reads=['accall', 'rbc'], writes=['h2'])
                    S.op('pool', lambda e: e.tensor_scalar(out=h2[:, c, :], in0=h2[:, c, :], scalar1=fg[:, c:c + 1], scalar2=None, op0=ALU.mult), reads=['h2', 'fg'], writes=['h2'])
                S.dma_start('sp', o_n[:, :, t0:t0 + 512].rearrange("k p t -> p k t"), h2[:], reads=['h2'], is_output=True)
            _barrier(S)
        S.finish('sp')
    return nc, S


def _chunkT(a):
    T, Fd = a.shape
    return np.ascontiguousarray(a.T.reshape(Fd // 128, 128, T))


def _col(v):
    return np.ascontiguousarray(v.reshape(-1, 128).T)


def _slab(w):
    K, N = w.shape
    return np.ascontiguousarray(w.reshape(K // 128, 128, N // 128, 128).transpose(2, 1, 0, 3))


def _pkn(w):
    K, N = w.shape
    return np.ascontiguousarray(w.reshape(K // 128, 128, N).transpose(1, 0, 2))


_PROG = {}


def _get_prog(name):
    if name not in _PROG:
        _PROG[name] = build_A()[0] if name == 'A' else build_B()[0]
    return _PROG[name]


def _a_inputs(l, b, g, xTb, I):
    f = np.float32
    d = {}
    d['xT'] = xTb
    d['cst'] = _col(I['c'][b])
    d['adaw'] = _slab(I['ada_w'][l][:, :2048])
    d['adab'] = _col(I['ada_b'][l][:2048])
    d['n1g'] = _col(I['norm1_g'][l])
    d['wsel'] = _pkn(I['w_in'][l][:, _sel_cols(g)])
    w1 = I['nsa_cmp_w1'][l]
    d['w1d'] = np.ascontiguousarray(
        np.stack([w1[kv].reshape(16, 2, 64, 2, 128).transpose(3, 1, 2, 0, 4).reshape(2, 128, 16, 128) for kv in range(2)]).reshape(4, 128, 16, 128))
    d['pos2'] = np.ascontiguousarray(I['nsa_cmp_pos'][l].reshape(2, 16, 2, 64).transpose(2, 3, 0, 1).reshape(128, 2, 16))
    d['w2d'] = np.ascontiguousarray(I['nsa_cmp_w2'][l].reshape(2, 2, 128, 64).transpose(2, 0, 1, 3))
    for k in ('triq', 'tri2q', 'ident', 'id4', 'keep', 'add', 'ov', 'cm'):
        d[k] = _CONST[k]
    G0 = 8 * g
    lr = I['ssm_lam_re'][l][G0:G0 + 8]; li = I['ssm_lam_im'][l][G0:G0 + 8]; ldt = I['ssm_log_dt'][l][G0:G0 + 8]
    ldt_full = np.repeat(ldt[:, None], 64, axis=1)
    d['s_lr'] = np.ascontiguousarray(np.tile(lr.reshape(1, 512), (128, 1)))
    d['s_li'] = np.ascontiguousarray(np.tile(li.reshape(1, 512), (128, 1)))
    d['s_ldt'] = np.ascontiguousarray(np.tile(ldt_full.reshape(1, 512), (128, 1)))
    zb_re = np.zeros((8, 16, 8, 64), f); zb_im = np.zeros((8, 16, 8, 64), f)
    zc_re = np.zeros((2, 64, 4, 8, 16), f); zc_im = np.zeros((2, 64, 4, 8, 16), f)
    for gl in range(8):
        zb_re[gl, :, gl, :] = I['ssm_b_re'][l][G0 + gl].T
        zb_im[gl, :, gl, :] = I['ssm_b_im'][l][G0 + gl].T
        j, gl2 = gl // 2, gl % 2
        zc_re[gl2, :, j, gl, :] = I['ssm_c_re'][l][G0 + gl].T
        zc_im[gl2, :, j, gl, :] = I['ssm_c_im'][l][G0 + gl].T
    d['s_bre'] = zb_re.reshape(128, 512); d['s_bim'] = zb_im.reshape(128, 512)
    d['s_cre'] = zc_re.reshape(128, 4, 128); d['s_cim'] = zc_im.reshape(128, 4, 128)
    d['s_lrc'] = np.ascontiguousarray(lr.reshape(4, 2, 64).transpose(1, 2, 0).reshape(128, 4))
    d['s_lic'] = np.ascontiguousarray(li.reshape(4, 2, 64).transpose(1, 2, 0).reshape(128, 4))
    d['s_ldtc'] = np.ascontiguousarray(ldt_full.reshape(4, 2, 64).transpose(1, 2, 0).reshape(128, 4))
    d['s_d'] = np.ascontiguousarray(I['ssm_d'][l][G0:G0 + 8].reshape(128, 1))
    d['s_iota'] = np.ascontiguousarray(np.tile(np.arange(512, dtype=f)[None], (128, 1)))
    return {k: np.ascontiguousarray(v, dtype=f) for k, v in d.items()}


def _run_A(l, xT_all, I):
    nc = _get_prog('A')
    maps = []
    for core in range(8):
        b, g = core // 4, core % 4
        maps.append(_a_inputs(l, b, g, xT_all[b], I))
    res = run_bass_kernel_spmd(nc, maps, core_ids=list(range(8))).results
    attn = np.zeros((2, SEQ, 1024), np.float32)
    ssmT = np.zeros((2, 512, SEQ), np.float32)
    for core in range(8):
        b, g = core // 4, core % 4
        attn[b][:, g * 256:(g + 1) * 256] = res[core]['o_attn']
        ssmT[b][g * 128:(g + 1) * 128, :] = res[core]['o_ssm']
    return attn, ssmT


def _run_B(l, xT_all, attn, ssmT, I):
    nc = _get_prog('B')
    f = np.float32
    o4 = 1024 + 1536 + 48 + 512
    common = dict(
        adaw=_slab(I['ada_w'][l]), adab=_col(I['ada_b'][l]), n1g=_col(I['norm1_g'][l]), n2g=_col(I['norm2_g'][l]),
        fing=_col(I['final_g']), wmg=_slab(I['w_in'][l][:, o4:o4 + 2048]), wglu=_slab(I['ssm_w_glu'][l]),
        wnso=_slab(I['nsa_w_o'][l]), wsso=_slab(I['ssm_w_o'][l]), wout=_slab(I['w_out'][l]),
        wr=_pkn(np.concatenate([I['moe_w_group'][l], I['moe_w_expert'][l]], axis=1)),
        br=np.concatenate([I['moe_b_group'][l], I['moe_b_expert'][l]])[None, :],
        mwg=I['moe_w_gate'][l], mwu=I['moe_w_up'][l], mwd=I['moe_w_down'][l],
        ident=_CONST['ident'], Rsel=np.repeat(np.eye(32, dtype=f), 128, axis=1),
    )
    common = {k: np.ascontiguousarray(v, dtype=f) for k, v in common.items()}
    maps = []
    for core in range(8):
        b, qt = core // 4, core % 4
        tok = slice(qt * NTOK, (qt + 1) * NTOK)
        d = dict(common)
        d['xT'] = np.ascontiguousarray(xT_all[b][:, :, tok])
        d['aT'] = _chunkT(attn[b][tok])
        d['sT'] = np.ascontiguousarray(ssmT[b][:, tok].reshape(4, 128, NTOK))
        d['cst'] = _col(I['c'][b]).astype(f)
        maps.append(d)
    res = run_bass_kernel_spmd(nc, maps, core_ids=list(range(8))).results
    x_new = [np.zeros((8, 128, SEQ), f) for _ in range(2)]
    x_nrm = [np.zeros((8, 128, SEQ), f) for _ in range(2)]
    for core in range(8):
        b, qt = core // 4, core % 4
        x_new[b][:, :, qt * NTOK:(qt + 1) * NTOK] = res[core]['o_x']
        x_nrm[b][:, :, qt * NTOK:(qt + 1) * NTOK] = res[core]['o_n']
    return x_new, x_nrm


def kernel(**inputs):
    I = {k: np.asarray(v, dtype=np.float32) for k, v in inputs.items()}
    xT_all = [_chunkT(I['x'][b]) for b in range(2)]
    x_nrm = None
    for l in range(2):
        attn, ssmT = _run_A(l, xT_all, I)
        xT_all, x_nrm = _run_B(l, xT_all, attn, ssmT, I)
    out = np.stack([x_nrm[b].reshape(1024, SEQ).T for b in range(2)])
    return np.ascontiguousarray(out, dtype=np.float32)
```

```python
import contextlib
import numpy as np
import concourse.bass as bass
import concourse.mybir as mybir
from concourse.bass_utils import run_bass_kernel_spmd

F32 = mybir.dt.float32
I32 = mybir.dt.int32
AF = mybir.ActivationFunctionType
ALU = mybir.AluOpType
AX = mybir.AxisListType

D = 1024
SEQ = 8192
NEGM = -1.0e5
EPS = 1e-6
TWO_PI = float(2 * np.pi)


class Sch:
    EPOCH = 30000
    NDMA = 24

    def __init__(self, nc):
        self.nc = nc
        self.eng = {'pe': nc.tensor, 'dve': nc.vector, 'act': nc.scalar, 'pool': nc.gpsimd, 'sp': nc.sync}
        self.sem = {}
        self.cnt = {}
        self.nsem = 0
        for k in self.eng:
            self._new_sem(k)
        self.seen = {k: {} for k in self.eng}
        self.dma = []
        for i in range(self.NDMA):
            self.dma.append([nc.alloc_semaphore(name=f"dma{i}"), 0, f"dma{i}"])
        self.dma_rr = 0
        self.last_w = {}
        self.readers = {}
        self.out_tokens = []
        self.n_inst = 0

    def _new_sem(self, k):
        self.nsem += 1
        self.sem[k] = (self.nc.alloc_semaphore(name=f"e_{k}_{self.nsem}"), f"e_{k}_{self.nsem}")
        self.cnt[k] = 0

    def _wait(self, e, tok):
        h, sid, val, owner = tok
        if owner == 'pe' and e == 'pe':
            return
        if self.seen[e].get(sid, 0) >= val:
            return
        self.eng[e].wait_ge(h, val)
        self.seen[e][sid] = val
        self.n_inst += 1

    def _deps(self, e, reads, writes):
        toks = []
        for b in reads:
            t = self.last_w.get(b)
            if t is not None:
                toks.append(t)
        for b in writes:
            t = self.last_w.get(b)
            if t is not None:
                toks.append(t)
            toks.extend(self.readers.get(b, ()))
        for t in toks:
            self._wait(e, t)

    def _commit(self, tok, reads, writes):
        for b in reads:
            if b not in writes:
                self.readers.setdefault(b, []).append(tok)
        for b in writes:
            self.last_w[b] = tok
            self.readers[b] = []

    def op(self, e, fn, reads=(), writes=()):
        self._deps(e, reads, writes)
        if self.cnt[e] >= self.EPOCH:
            self._new_sem(e)
        inst = fn(self.eng[e])
        h, sid = self.sem[e]
        self.cnt[e] += 1
        inst.then_inc(h, 1)
        tok = (h, sid, self.cnt[e], e)
        self._commit(tok, reads, writes)
        self.n_inst += 1
        return tok

    def dma(self_, *a, **k):
        raise NotImplementedError

    def dma_start(self, e, out, in_, reads=(), writes=(), is_output=False, **kw):
        self._deps(e, reads, writes)
        slot = self.dma[self.dma_rr]
        self.dma_rr = (self.dma_rr + 1) % self.NDMA
        h, val, sid = slot
        if val > 0:
            self._wait(e, (h, sid, val, 'dma'))
        inst = self.eng[e].dma_start(out=out, in_=in_, **kw)
        slot[1] = val + 16
        inst.then_inc(h, 16)
        tok = (h, sid, val + 16, 'dma')
        self._commit(tok, reads, writes)
        if is_output:
            self.out_tokens.append(tok)
        self.n_inst += 1
        return tok

    def finish(self, e='sp'):
        for t in self.out_tokens:
            self._wait(e, t)
        for k in self.eng:
            if k != e and self.cnt[k] > 0:
                h, sid = self.sem[k]
                self._wait(e, (h, sid, self.cnt[k], k))


def _const_tables():
    q = np.arange(128)[:, None]
    k = np.arange(128)[None, :]
    triq = np.where(k <= q, 0.0, NEGM).astype(np.float32)
    tri2q = np.where(k > q, 0.0, NEGM).astype(np.float32)
    ident = np.eye(128, dtype=np.float32)
    id4 = np.tile(ident, (1, 4))
    keep = np.ones((128, 256), np.float32)
    add = np.zeros((128, 256), np.float32)
    for qq in range(128):
        half = qq // 64
        for c in range(256):
            rel = c - 128
            if rel > half:
                keep[qq, c] = 0.0; add[qq, c] = -1.0
            elif rel == half:
                keep[qq, c] = 0.0; add[qq, c] = 1.0e4
            elif rel == half - 1:
                keep[qq, c] = 0.0; add[qq, c] = 2.0e4
    ov = np.zeros((512, 128), np.float32)
    for m in range(1, 512):
        n = m - 1
        for s in range(128):
            if 16 * n < 64 * s + 64 and 16 * n + 32 > 64 * s:
                ov[m, s] = 1.0
    tabs = []
    tab_index = {}
    keymap = {}
    for i in range(64):
        nb = (8 * i + 7) // 128 + 1
        for jb in range(nb):
            if jb >= 1 and 128 * jb + 127 <= 8 * i - 1:
                continue
            m = 128 * jb + np.arange(128)[None, :]
            t = 128 * i + np.arange(128)[:, None]
            vis = (m >= 1) & (16 * (m - 1) + 31 <= t)
            tab = np.where(vis, 0.0, NEGM).astype(np.float32)
            key = tab.tobytes()
            if key not in tab_index:
                tab_index[key] = len(tabs)
                tabs.append(tab)
            keymap[(i, jb)] = tab_index[key]
    return dict(triq=triq, tri2q=tri2q, ident=ident, id4=id4, keep=keep, add=add,
                ov=ov.reshape(4, 128, 128).transpose(1, 0, 2).copy(), cm=np.stack(tabs)), keymap


_CONST, _CMKEY = _const_tables()
NQ = 256
NCOL = 908
C_Q = 0
C_KC = 256
C_VC = 384
C_KS = 512
C_KW = 576
C_TM = 640
C_U = 780


def _sel_cols(g):
    o1 = 1024
    o2 = o1 + 6 * 256
    o3 = o2 + 48
    cols = list(range(g * 256, g * 256 + 256))
    kc = list(range(o1 + 0 * 256 + g * 64, o1 + 0 * 256 + g * 64 + 64))
    vc = list(range(o1 + 1 * 256 + g * 64, o1 + 1 * 256 + g * 64 + 64))
    ks = list(range(o1 + 2 * 256 + g * 64, o1 + 2 * 256 + g * 64 + 64))
    vs = list(range(o1 + 3 * 256 + g * 64, o1 + 3 * 256 + g * 64 + 64))
    kw = list(range(o1 + 4 * 256 + g * 64, o1 + 4 * 256 + g * 64 + 64))
    vw = list(range(o1 + 5 * 256 + g * 64, o1 + 5 * 256 + g * 64 + 64))
    gt = [o2 + br * 16 + g * 4 + h for br in range(3) for h in range(4)]
    u = list(range(o3 + g * 128, o3 + g * 128 + 128))
    cols = cols + kc + kc + vc + vc + ks + kw + vs + vw + gt + u
    assert len(cols) == NCOL
    return np.array(cols)


def build_A(n_tiles=SEQ // 512, debug=False):
    nc = bass.Bass("TRN2", target_bir_lowering=False)

    def din(name, shape):
        return nc.dram_tensor(name, list(shape), F32, kind="ExternalInput").ap()

    xT = din("xT", [8, 128, SEQ])
    cst = din("cst", [128, 8])
    adaw = din("adaw", [16, 128, 8, 128])
    adab = din("adab", [128, 16])
    n1g = din("n1g", [128, 8])
    wsel = din("wsel", [128, 8, NCOL])
    w1d = din("w1d", [4, 128, 16, 128])
    pos2 = din("pos2", [128, 2, 16])
    w2d = din("w2d", [128, 2, 2, 64])
    c_triq = din("triq", [128, 128]); c_tri2q = din("tri2q", [128, 128])
    c_ident = din("ident", [128, 128]); c_id4 = din("id4", [128, 512])
    c_keep = din("keep", [128, 256]); c_add = din("add", [128, 256])
    c_ov = din("ov", [128, 4, 128])
    c_cm = din("cm", list(_CONST['cm'].shape))
    s_lr = din("s_lr", [128, 512]); s_li = din("s_li", [128, 512]); s_ldt = din("s_ldt", [128, 512])
    s_bre = din("s_bre", [128, 512]); s_bim = din("s_bim", [128, 512])
    s_lrc = din("s_lrc", [128, 4]); s_lic = din("s_lic", [128, 4]); s_ldtc = din("s_ldtc", [128, 4])
    s_cre = din("s_cre", [128, 4, 128]); s_cim = din("s_cim", [128, 4, 128])
    s_d = din("s_d", [128, 1])
    s_iota = din("s_iota", [128, 512])
    o_attn = nc.dram_tensor("o_attn", [SEQ, 256], F32, kind="ExternalOutput").ap()
    o_ssm = nc.dram_tensor("o_ssm", [128, SEQ], F32, kind="ExternalOutput").ap()
    o_dbg = nc.dram_tensor("o_dbg", [64, 128, 4, 128], F32, kind="ExternalOutput").ap() if debug else None

    S = Sch(nc)
    with contextlib.ExitStack() as st:
        def sb(name, shape, dt=F32):
            return st.enter_context(nc.sbuf_tensor(name, list(shape), dt))

        def ps(name, shape):
            return st.enter_context(nc.psum_tensor(name, list(shape), F32))

        W = sb("W", [128, 8, NCOL])
        biasrow = sb("biasrow", [1, NCOL])
        ones = sb("ones", [128, 128]); zer = sb("zer", [128, 512])
        triq = sb("triq_s", [128, 128]); tri2q = sb("tri2q_s", [128, 128])
        ident = sb("ident_s", [128, 128]); id4 = sb("id4_s", [128, 512])
        keep = sb("keep_s", [128, 256]); addt = sb("add_s", [128, 256]); ov = sb("ov_s", [128, 4, 128])
        w1s = [sb(f"w1s{i}", [128, 16, 128]) for i in range(2)]
        w2s = sb("w2s", [128, 2, 2, 64]); b1 = sb("b1", [128, 4]); pos2s = sb("pos2s", [128, 2, 16])
        BBre = sb("BBre", [128, 512]); BBim = sb("BBim", [128, 512])
        CCre = sb("CCre", [128, 4, 128]); CCim = sb("CCim", [128, 4, 128])
        cosT = sb("cosT", [128, 4, 256]); sinT = sb("sinT", [128, 4, 256])
        rcolS = sb("rcolS", [128, 4]); thc = sb("thc", [128, 4]); dcol = sb("dcol", [128, 1])
        carry_re = sb("carry_re", [128, 4]); carry_im = sb("carry_im", [128, 4])
        erc = sb("erc", [128, 4]); eic = sb("eic", [128, 4])

        pA = [ps(f"pA{i}", [128, 512]) for i in range(2)]
        pS = [ps(f"pS{i}", [128, 512]) for i in range(2)]
        pMb = ps("pMb", [128, 512])
        pOf = [ps(f"pO{i}", [128, 512]) for i in range(2)]
        pO = [t_[:, 0:260].rearrange("p (a b) -> p a b", a=4) for t_ in pOf]
        pI = ps("pI", [128, 4, 128])
        rot = {'pA': 0, 'pS': 0, 'pO': 0, 'pT': 0, 'xsq': 0, 'ne': 0, 'cmt': 0, 'w1s': 0, 'aout': 0, 'yT': 0, 'pM': 0}

        def nxt(k, n=2):
            v = rot[k]; rot[k] = (v + 1) % n
            return v

        def mm(out, lhsT, rhs, start, stop, reads, writes, skip=False):
            if skip:
                S.op('pe', lambda e: e.matmul(out, lhsT=lhsT, rhs=rhs, start=start, stop=stop, skip_group_check=True), reads=reads, writes=writes)
            else:
                S.op('pe', lambda e: e.matmul(out, lhsT=lhsT, rhs=rhs, start=start, stop=stop), reads=reads, writes=writes)

        S.dma_start('sp', W[:], wsel, writes=['W'])
        for nm, t, src in (('triq', triq, c_triq), ('tri2q', tri2q, c_tri2q), ('ident', ident, c_ident), ('id4', id4, c_id4),
                           ('keep', keep, c_keep), ('add', addt, c_add), ('ov', ov, c_ov), ('w2s', w2s, w2d), ('pos2s', pos2s, pos2)):
            S.dma_start('sp', t[:], src, writes=[nm])
        S.op('pool', lambda e: e.memset(ones[:], 1.0), writes=['ones'])
        S.op('pool', lambda e: e.memset(zer[:], 0.0), writes=['zer'])
        S.op('pool', lambda e: e.memset(carry_re[:], 0.0), writes=['carry'])
        S.op('pool', lambda e: e.memset(carry_im[:], 0.0), writes=['carry'])

        with contextlib.ExitStack() as st2:
            def sb2(name, shape, dt=F32):
                return st2.enter_context(nc.sbuf_tensor(name, list(shape), dt))
            cs = sb2("cs", [128, 8]); sg = sb2("sg", [128, 8])
            adat = [sb2(f"adat{i}", [128, 8, 128]) for i in range(2)]
            adabs = sb2("adabs", [128, 16]); modc = sb2("modc", [128, 16]); n1 = sb2("n1", [128, 8]); scl = sb2("scl", [128, 8])
            S.dma_start('sp', cs[:], cst, writes=['cs'])
            S.dma_start('sp', adabs[:], adab, writes=['adabs'])
            S.dma_start('sp', n1[:], n1g, writes=['n1'])
            S.op('act', lambda e: e.activation(out=sg[:], in_=cs[:], func=AF.Sigmoid), reads=['cs'], writes=['sg'])
            S.op('dve', lambda e: e.tensor_tensor(out=cs[:], in0=cs[:], in1=sg[:], op=ALU.mult), reads=['cs', 'sg'], writes=['cs'])
            for j in range(16):
                a = adat[j % 2]; an = f"adat{j % 2}"
                S.dma_start('sp', a[:], adaw[j], writes=[an])
                p = pA[nxt('pA')]; pn = f"pA{rot['pA'] ^ 1}"
                for kc in range(8):
                    mm(p[:, 0:1], a[:, kc, :], cs[:, kc:kc + 1], kc == 0, kc == 7, [an, 'cs'], [pn])
                S.op('dve', lambda e: e.tensor_tensor(out=modc[:, j:j + 1], in0=p[:, 0:1], in1=adabs[:, j:j + 1], op=ALU.add),
                     reads=[pn, 'adabs'], writes=['modc'])
            for (c0, c1) in ((0, 512), (512, NCOL)):
                p = pA[nxt('pA')]; pn = f"pA{rot['pA'] ^ 1}"
                for kc in range(8):
                    mm(p[0:1, 0:c1 - c0], modc[:, kc:kc + 1], W[:, kc, c0:c1], kc == 0, kc == 7, ['modc', 'W'], [pn])
                S.op('act', lambda e: e.activation(out=biasrow[0:1, c0:c1], in_=p[0:1, 0:c1 - c0], func=AF.Copy), reads=[pn], writes=['biasrow'])
            S.op('dve', lambda e: e.tensor_scalar(out=scl[:], in0=modc[:, 8:16], scalar1=1.0, scalar2=None, op0=ALU.add), reads=['modc'], writes=['scl'])
            S.op('dve', lambda e: e.tensor_tensor(out=scl[:], in0=scl[:], in1=n1[:], op=ALU.mult), reads=['scl', 'n1'], writes=['scl'])
            for kc in range(8):
                S.op('dve' if kc % 2 else 'pool', lambda e: e.tensor_scalar(out=W[:, kc, :], in0=W[:, kc, :], scalar1=scl[:, kc:kc + 1], scalar2=None, op0=ALU.mult),
                     reads=['W', 'scl'], writes=['W'])
            for c4 in range(4):
                kv = c4 // 2
                w = w1s[c4 % 2]; wn = f"w1s{c4 % 2}"
                S.dma_start('sp', w[:], w1d[c4], writes=[wn])
                p = pA[nxt('pA')]; pn = f"pA{rot['pA'] ^ 1}"
                for lp in range(16):
                    mm(p[:, 0:1], w[:, lp, :], pos2s[:, kv, lp:lp + 1], lp == 0, lp == 15, [wn, 'pos2s'], [pn])
                S.op('act', lambda e: e.activation(out=b1[:, c4:c4 + 1], in_=p[:, 0:1], func=AF.Copy), reads=[pn], writes=['b1'])

            t = [sb2(f"pt{i}", [128, 512]) for i in range(10)]
            ti = sb2("pti", [128, 512], I32)
            for nm, tt, src in (('t0', t[0], s_lr), ('t1', t[1], s_li), ('t2', t[2], s_ldt), ('t8', t[8], s_bre), ('t9', t[9], s_bim)):
                S.dma_start('sp', tt[:], src, writes=[nm])
            S.dma_start('sp', CCre[:], s_cre, writes=['CC']); S.dma_start('sp', CCim[:], s_cim, writes=['CC'])
            S.dma_start('sp', dcol[:], s_d, writes=['dcol'])

            def dv(fn, reads, writes, e='dve'):
                S.op(e, fn, reads=reads, writes=writes)

            def sincos(out_sin, out_cos, arg, n, names, onames):
                for (o, shift) in ((out_sin, 0.5), (out_cos, 0.75)):
                    dv(lambda e: e.tensor_scalar(out=t[6][:, 0:n], in0=arg, scalar1=float(1 / TWO_PI), scalar2=shift, op0=ALU.mult, op1=ALU.add), names, ['t6'])
                    dv(lambda e: e.tensor_copy(out=ti[:, 0:n], in_=t[6][:, 0:n]), ['t6'], ['ti'])
                    dv(lambda e: e.tensor_copy(out=t[7][:, 0:n], in_=ti[:, 0:n]), ['ti'], ['t7'])
                    dv(lambda e: e.tensor_tensor(out=t[6][:, 0:n], in0=t[6][:, 0:n], in1=t[7][:, 0:n], op=ALU.subtract), ['t6', 't7'], ['t6'])
                    dv(lambda e: e.tensor_scalar(out=t[7][:, 0:n], in0=t[6][:, 0:n], scalar1=0.0, scalar2=None, op0=ALU.is_lt), ['t6'], ['t7'])
                    dv(lambda e: e.tensor_tensor(out=t[6][:, 0:n], in0=t[6][:, 0:n], in1=t[7][:, 0:n], op=ALU.add), ['t6', 't7'], ['t6'])
                    dv(lambda e: e.tensor_scalar(out=t[6][:, 0:n], in0=t[6][:, 0:n], scalar1=TWO_PI, scalar2=float(np.pi), op0=ALU.mult, op1=ALU.subtract), ['t6'], ['t6'])
                    dv(lambda e: e.tensor_scalar(out=t[6][:, 0:n], in0=t[6][:, 0:n], scalar1=float(np.pi), scalar2=float(-np.pi), op0=ALU.min, op1=ALU.max), ['t6'], ['t6'])
                    S.op('act', lambda e: e.activation(out=o, in_=t[6][:, 0:n], func=AF.Sin), reads=['t6'], writes=onames + ['trig'])

            S.op('act', lambda e: e.activation(out=t[2][:], in_=t[2][:], func=AF.Exp), reads=['t2'], writes=['t2'])
            dv(lambda e: e.tensor_tensor(out=t[3][:], in0=t[0][:], in1=t[2][:], op=ALU.mult), ['t0', 't2'], ['t3'])
            S.op('act', lambda e: e.activation(out=t[3][:], in_=t[3][:], func=AF.Exp), reads=['t3'], writes=['t3'])
            dv(lambda e: e.tensor_tensor(out=t[4][:], in0=t[1][:], in1=t[2][:], op=ALU.mult), ['t1', 't2'], ['t4'])
            sincos(t[5][:], t[2][:], t[4][:], 512, ['t4'], ['t5', 't2'])
            dv(lambda e: e.tensor_tensor(out=t[5][:], in0=t[5][:], in1=t[3][:], op=ALU.mult), ['t5', 't3', 'trig'], ['t5'])
            dv(lambda e: e.tensor_tensor(out=t[2][:], in0=t[2][:], in1=t[3][:], op=ALU.mult), ['t2', 't3', 'trig'], ['t2'])
            dv(lambda e: e.tensor_scalar(out=t[2][:], in0=t[2][:], scalar1=-1.0, scalar2=None, op0=ALU.add), ['t2'], ['t2'])
            dv(lambda e: e.tensor_tensor(out=t[3][:], in0=t[0][:], in1=t[0][:], op=ALU.mult), ['t0'], ['t3'])
            dv(lambda e: e.tensor_tensor(out=t[4][:], in0=t[1][:], in1=t[1][:], op=ALU.mult), ['t1'], ['t4'])
            dv(lambda e: e.tensor_tensor(out=t[3][:], in0=t[3][:], in1=t[4][:], op=ALU.add), ['t3', 't4'], ['t3'])
            dv(lambda e: e.reciprocal(out=t[3][:], in_=t[3][:]), ['t3'], ['t3'])
            dv(lambda e: e.tensor_tensor(out=t[6][:], in0=t[2][:], in1=t[0][:], op=ALU.mult), ['t2', 't0', 'trig'], ['t6'])
            dv(lambda e: e.tensor_tensor(out=t[4][:], in0=t[5][:], in1=t[1][:], op=ALU.mult), ['t5', 't1'], ['t4'])
            dv(lambda e: e.tensor_tensor(out=t[6][:], in0=t[6][:], in1=t[4][:], op=ALU.add), ['t6', 't4'], ['t6'])
            dv(lambda e: e.tensor_tensor(out=t[6][:], in0=t[6][:], in1=t[3][:], op=ALU.mult), ['t6', 't3'], ['t6'])
            dv(lambda e: e.tensor_tensor(out=t[7][:], in0=t[5][:], in1=t[0][:], op=ALU.mult), ['t5', 't0'], ['t7'])
            dv(lambda e: e.tensor_tensor(out=t[4][:], in0=t[2][:], in1=t[1][:], op=ALU.mult), ['t2', 't1'], ['t4'])
            dv(lambda e: e.tensor_tensor(out=t[7][:], in0=t[7][:], in1=t[4][:], op=ALU.subtract), ['t7', 't4'], ['t7'])
            dv(lambda e: e.tensor_tensor(out=t[7][:], in0=t[7][:], in1=t[3][:], op=ALU.mult), ['t7', 't3'], ['t7'])
            dv(lambda e: e.tensor_tensor(out=BBre[:], in0=t[6][:], in1=t[8][:], op=ALU.mult), ['t6', 't8'], ['BB'])
            dv(lambda e: e.tensor_tensor(out=t[4][:], in0=t[7][:], in1=t[9][:], op=ALU.mult), ['t7', 't9'], ['t4'])
            dv(lambda e: e.tensor_tensor(out=BBre[:], in0=BBre[:], in1=t[4][:], op=ALU.subtract), ['BB', 't4'], ['BB'])
            dv(lambda e: e.tensor_tensor(out=BBim[:], in0=t[6][:], in1=t[9][:], op=ALU.mult), ['t6', 't9'], ['BB'])
            dv(lambda e: e.tensor_tensor(out=t[4][:], in0=t[7][:], in1=t[8][:], op=ALU.mult), ['t7', 't8'], ['t4'])
            dv(lambda e: e.tensor_tensor(out=BBim[:], in0=BBim[:], in1=t[4][:], op=ALU.add), ['BB', 't4'], ['BB'])
            dv(lambda e: e.tensor_scalar(out=CCim[:], in0=CCim[:], scalar1=-1.0, scalar2=None, op0=ALU.mult), ['CC'], ['CC'])
            lrc = sb2("lrc", [128, 4]); lic = sb2("lic", [128, 4]); dtc = sb2("dtc", [128, 4])
            S.dma_start('sp', lrc[:], s_lrc, writes=['lrc']); S.dma_start('sp', lic[:], s_lic, writes=['lic']); S.dma_start('sp', dtc[:], s_ldtc, writes=['dtc'])
            S.op('act', lambda e: e.activation(out=dtc[:], in_=dtc[:], func=AF.Exp), reads=['dtc'], writes=['dtc'])
            dv(lambda e: e.tensor_tensor(out=rcolS[:], in0=lrc[:], in1=dtc[:], op=ALU.mult), ['lrc', 'dtc'], ['rcolS'])
            S.op('act', lambda e: e.activation(out=rcolS[:], in_=rcolS[:], func=AF.Exp), reads=['rcolS'], writes=['rcolS'])
            dv(lambda e: e.tensor_tensor(out=thc[:], in0=lic[:], in1=dtc[:], op=ALU.mult), ['lic', 'dtc'], ['thc'])
            sincos(eic[:], erc[:], thc[:], 4, ['thc'], ['eirc'])
            S.dma_start('sp', t[0][:], s_iota, writes=['t0'])
            for j in range(4):
                dv(lambda e: e.tensor_scalar(out=t[1][:, 0:256], in0=t[0][:, 0:256], scalar1=thc[:, j:j + 1], scalar2=None, op0=ALU.mult), ['t0', 'thc'], ['t1'])
                sincos(sinT[:, j, :], cosT[:, j, :], t[1][:, 0:256], 256, ['t1'], ['cossin'])
        _barrier(S)
        KT = sb("KT", [64, SEQ])
        kwr = sb("kwr", [64, 1024])
        vslc = sb("vslc", [128, 64, 65])
        vwr = sb("vwr", [128, 8, 65])
        kbuf = sb("kbuf", [128, 528]); vbuf = sb("vbuf", [128, 528])
        kcT = sb("kcT", [64, 512]); vc = sb("vc", [128, 4, 65])
        qT = sb("qT", [64, 4, 4, 128])
        uT = sb("uT", [128, 512])
        gts = sb("gts", [128, 4, 12])
        rcol = sb("rcol", [128, 4])
        xt = sb("xt", [128, 8, 512])
        xsq = [sb(f"xsq{i}", [128, 512]) for i in range(2)]
        sqbc = sb("sqbc", [128, 512]); rbc = sb("rbc", [128, 512])
        hidk = sb("hidk", [128, 2, 32]); hidv = sb("hidv", [128, 2, 128])
        pT = [sb(f"pT{i}", [128, 512]) for i in range(2)]
        negexp = [sb(f"negexp{i}", [128, 128]) for i in range(2)]
        cmt = [sb(f"cmt{i}", [128, 128]) for i in range(2)]
        imp = sb("imp", [128, 128]); imp2 = sb("imp2", [128, 128]); wk = sb("wk", [128, 128]); negm = sb("negm", [128, 128])
        m8 = sb("m8", [128, 8]); m8b = sb("m8b", [128, 8])
        den = sb("den", [128, 3, 4]); coef = sb("coef", [128, 3, 4])
        osb = [sb(f"osb{i}", [128, 4, 65]) for i in range(3)]
        aout = [sb(f"aout{i}", [128, 256]) for i in range(2)]
        cin_re = sb("cin_re", [128, 1]); cin_im = sb("cin_im", [128, 1]); ctmp = sb("ctmp", [128, 2])
        sw = [sb(f"sw{i}", [128, 256]) for i in range(8)]
        hre = sb("hre", [128, 4, 256]); him = sb("him", [128, 4, 256])
        yT = [sb(f"yT{i}", [128, 512]) for i in range(2)]

        S.op('pool', lambda e: e.memset(vslc[:], 1.0), writes=['vslc'])
        S.op('pool', lambda e: e.memset(vwr[:], 1.0), writes=['vwr'])
        S.op('pool', lambda e: e.memset(vc[:], 0.0), writes=['vc'])
        S.op('pool', lambda e: e.memset(vc[:, :, 64:65], 1.0), writes=['vc'])
        S.op('pool', lambda e: e.memset(kbuf[:], 0.0), writes=['kbuf'])
        S.op('pool', lambda e: e.memset(vbuf[:], 0.0), writes=['vbuf'])
        S.op('pool', lambda e: e.memset(kcT[:], 0.0), writes=['kcT'])

        _barrier(S)
        TRIG = ['trig']

        def evac_mul(dst, src, rb, reads, writes, e='dve'):
            S.op(e, lambda en: en.tensor_tensor(out=dst, in0=src, in1=rb, op=ALU.mult), reads=reads, writes=writes)

        for I in range(n_tiles):
            t0 = I * 512
            S.dma_start('sp', xt[:], xT[:, :, t0:t0 + 512].rearrange("k p t -> p k t"), writes=['xt'])
            pq = pA[nxt('pA')]; pqn = f"pA{rot['pA'] ^ 1}"
            for kc in range(8):
                xi = nxt('xsq'); xs = xsq[xi]; xn = f"xsq{xi}"
                S.op('act', lambda e: e.activation(out=xs[:], in_=xt[:, kc, :], func=AF.Square), reads=['xt'], writes=[xn])
                mm(pq[:], ones[:], xs[:], kc == 0, kc == 7, ['ones', xn], [pqn])
            S.op('act', lambda e: e.activation(out=sqbc[:], in_=pq[:], func=AF.Sqrt, scale=1.0 / D, bias=EPS), reads=[pqn], writes=['sqbc'])
            S.op('dve', lambda e: e.reciprocal(out=rbc[:], in_=sqbc[:]), reads=['sqbc'], writes=['rbc'])
            pr = pA[nxt('pA')]; prn = f"pA{rot['pA'] ^ 1}"
            for ts in range(4):
                mm(pr[:, ts:ts + 1], rbc[0:1, ts * 128:(ts + 1) * 128], ones[0:1, 0:1], True, True, ['rbc', 'ones'], [prn])
            S.op('act', lambda e: e.activation(out=rcol[:], in_=pr[:, 0:4], func=AF.Copy), reads=[prn], writes=['rcol'])

            def fm(c0, M):
                p = pA[nxt('pA')]; pn = f"pA{rot['pA'] ^ 1}"
                for kc in range(8):
                    mm(p[0:M, :], W[:, kc, c0:c0 + M], xt[:, kc, :], kc == 0, False, ['W', 'xt'], [pn])
                mm(p[0:M, :], biasrow[0:1, c0:c0 + M], sqbc[0:1, :], False, True, ['biasrow', 'sqbc'], [pn])
                return p, pn
            for h in range(4):
                p, pn = fm(C_Q + 64 * h, 64)
                S.op('dve', lambda e: e.tensor_tensor(out=qT[:, :, h, :], in0=p[0:64, :].rearrange("p (a b) -> p a b", a=4),
                                                      in1=rbc[0:64, :].rearrange("p (a b) -> p a b", a=4), op=ALU.mult),
                     reads=[pn, 'rbc'], writes=['qT'])
            for (buf, bn, c0) in ((kbuf, 'kbuf', C_KC), (vbuf, 'vbuf', C_VC)):
                S.op('pool', lambda e: e.tensor_copy(out=buf[:, 0:16], in_=buf[:, 512:528]), reads=[bn], writes=[bn])
                p, pn = fm(c0, 128)
                evac_mul(buf[0:64, 16:528], p[0:64, :], rbc[0:64, :], [pn, 'rbc'], [bn])
                evac_mul(buf[64:128, 15:527], p[64:128, :], rbc[64:128, :], [pn, 'rbc'], [bn])
            p, pn = fm(C_KS, 64)
            evac_mul(KT[:, t0:t0 + 512], p[0:64, :], rbc[0:64, :], [pn, 'rbc'], [f'KT{I}'])
            p, pn = fm(C_KW, 64)
            r0 = (I % 2) * 512
            evac_mul(kwr[:, r0:r0 + 512], p[0:64, :], rbc[0:64, :], [pn, 'rbc'], [f'kwr{I % 2}'])
            p, pn = fm(C_U, 128)
            evac_mul(uT[:], p[:], rbc[:], [pn, 'rbc'], ['uT'])
            for ts in range(4):
                blk = I * 4 + ts
                p = pA[nxt('pA')]; pn = f"pA{rot['pA'] ^ 1}"
                for kc in range(8):
                    mm(p[:, 0:140], xt[:, kc, ts * 128:(ts + 1) * 128], W[:, kc, C_TM:C_TM + 140], kc == 0, False, ['xt', 'W'], [pn])
                mm(p[:, 0:140], sqbc[0:1, ts * 128:(ts + 1) * 128], biasrow[0:1, C_TM:C_TM + 140], False, True, ['sqbc', 'biasrow'], [pn])
                S.op('dve', lambda e: e.tensor_scalar(out=vslc[:, blk, 0:64], in0=p[:, 0:64], scalar1=rcol[:, ts:ts + 1], scalar2=None, op0=ALU.mult),
                     reads=[pn, 'rcol'], writes=[f'vslc{blk}'])
                S.op('dve', lambda e: e.tensor_scalar(out=vwr[:, blk % 8, 0:64], in0=p[:, 64:128], scalar1=rcol[:, ts:ts + 1], scalar2=None, op0=ALU.mult),
                     reads=[pn, 'rcol'], writes=[f'vwr{blk % 8}'])
                S.op('act', lambda e: e.activation(out=gts[:, ts, :], in_=p[:, 128:140], func=AF.Sigmoid, scale=rcol[:, ts:ts + 1]),
                     reads=[pn, 'rcol'], writes=['gts'])

            sl0 = 32 * I
            jb_new = sl0 // 128
            pc0 = sl0 % 128
            S.op('pool', lambda e: e.memset(hidv[:], 0.0), writes=['hidv'])
            for c4 in range(4):
                kv, hc = c4 // 2, c4 % 2
                wi = nxt('w1s'); w = w1s[wi]; wn = f"w1s{wi}"
                S.dma_start('sp', w[:], w1d[c4], writes=[wn])
                buf, bn = (kbuf, 'kbuf') if kv == 0 else (vbuf, 'vbuf')
                p = pA[nxt('pA')]; pn = f"pA{rot['pA'] ^ 1}"
                for lp in range(16):
                    mm(p[:, 0:32], w[:, lp, :], buf[:, 2 * lp:2 * lp + 16 * 31 + 1:16], lp == 0, lp == 15, [wn, bn], [pn])
                dst = hidk[:, hc, :] if kv == 0 else hidv[:, hc, pc0:pc0 + 32]
                S.op('act', lambda e: e.activation(out=dst, in_=p[:, 0:32], func=AF.Gelu_apprx_tanh, bias=b1[:, c4:c4 + 1]),
                     reads=[pn, 'b1'], writes=['hidk' if kv == 0 else 'hidv'])
            p = pA[nxt('pA')]; pn = f"pA{rot['pA'] ^ 1}"
            for hc in range(2):
                mm(p[0:64, 0:32], w2s[:, 0, hc, :], hidk[:, hc, :], hc == 0, hc == 1, ['w2s', 'hidk'], [pn])
            S.op('act', lambda e: e.activation(out=kcT[:, sl0:sl0 + 32], in_=p[0:64, 0:32], func=AF.Copy), reads=[pn], writes=['kcT'])
            p = pA[nxt('pA')]; pn = f"pA{rot['pA'] ^ 1}"
            for hc in range(2):
                mm(p[:, 0:64], hidv[:, hc, :], w2s[:, 1, hc, :], hc == 0, hc == 1, ['w2s', 'hidv'], [pn])
            S.op('dve', lambda e: e.tensor_tensor(out=vc[:, jb_new, 0:64], in0=p[:, 0:64], in1=vc[:, jb_new, 0:64], op=ALU.add), reads=[pn, 'vc'], writes=['vc'])

            yi = nxt('yT'); yt = yT[yi]; yn = f"yT{yi}"
            for sc in range(2):
                c0 = sc * 256
                for j in range(4):
                    pre = pA[nxt('pA')]; pren = f"pA{rot['pA'] ^ 1}"
                    mm(pre[:, 0:256], BBre[:, j * 128:(j + 1) * 128], uT[:, c0:c0 + 256], True, True, ['BB', 'uT'], [pren])
                    mm(pre[:, 256:512], BBim[:, j * 128:(j + 1) * 128], uT[:, c0:c0 + 256], True, True, ['BB', 'uT'], [pren], skip=True)
                    cs_, sn_ = cosT[:, j, :], sinT[:, j, :]
                    bre, bim = pre[:, 0:256], pre[:, 256:512]
                    S.op('dve', lambda e: e.tensor_tensor(out=sw[0][:], in0=bre, in1=cs_, op=ALU.mult), reads=[pren] + TRIG, writes=['sw0'])
                    S.op('dve', lambda e: e.tensor_tensor(out=sw[1][:], in0=bim, in1=sn_, op=ALU.mult), reads=[pren] + TRIG, writes=['sw1'])
                    S.op('pool', lambda e: e.tensor_tensor(out=sw[0][:], in0=sw[0][:], in1=sw[1][:], op=ALU.add), reads=['sw0', 'sw1'], writes=['sw0'])
                    S.op('dve', lambda e: e.tensor_tensor(out=sw[2][:], in0=bim, in1=cs_, op=ALU.mult), reads=[pren] + TRIG, writes=['sw2'])
                    S.op('dve', lambda e: e.tensor_tensor(out=sw[3][:], in0=bre, in1=sn_, op=ALU.mult), reads=[pren] + TRIG, writes=['sw3'])
                    S.op('pool', lambda e: e.tensor_tensor(out=sw[2][:], in0=sw[2][:], in1=sw[3][:], op=ALU.subtract), reads=['sw2', 'sw3'], writes=['sw2'])
                    S.op('dve', lambda e: e.tensor_tensor(out=ctmp[:, 0:1], in0=carry_re[:, j:j + 1], in1=erc[:, j:j + 1], op=ALU.mult), reads=['carry'] + TRIG, writes=['ctmp'])
                    S.op('dve', lambda e: e.tensor_tensor(out=ctmp[:, 1:2], in0=carry_im[:, j:j + 1], in1=eic[:, j:j + 1], op=ALU.mult), reads=['carry'] + TRIG, writes=['ctmp'])
                    S.op('dve', lambda e: e.tensor_tensor(out=cin_re[:], in0=ctmp[:, 0:1], in1=ctmp[:, 1:2], op=ALU.subtract), reads=['ctmp'], writes=['cin'])
                    S.op('dve', lambda e: e.tensor_tensor(out=ctmp[:, 0:1], in0=carry_re[:, j:j + 1], in1=eic[:, j:j + 1], op=ALU.mult), reads=['carry'] + TRIG, writes=['ctmp'])
                    S.op('dve', lambda e: e.tensor_tensor(out=ctmp[:, 1:2], in0=carry_im[:, j:j + 1], in1=erc[:, j:j + 1], op=ALU.mult), reads=['carry'] + TRIG, writes=['ctmp'])
                    S.op('dve', lambda e: e.tensor_tensor(out=cin_im[:], in0=ctmp[:, 0:1], in1=ctmp[:, 1:2], op=ALU.add), reads=['ctmp'], writes=['cin'])
                    S.op('dve', lambda e: e.tensor_tensor_scan(out=sw[4][:], data0=rcolS[:, j:j + 1].to_broadcast([128, 256]), data1=sw[0][:],
                                                               initial=cin_re[:], op0=ALU.mult, op1=ALU.add), reads=['sw0', 'cin', 'rcolS'], writes=['sw4'])
                    S.op('dve', lambda e: e.tensor_tensor_scan(out=sw[5][:], data0=rcolS[:, j:j + 1].to_broadcast([128, 256]), data1=sw[2][:],
                                                               initial=cin_im[:], op0=ALU.mult, op1=ALU.add), reads=['sw2', 'cin', 'rcolS'], writes=['sw5'])
                    S.op('pool', lambda e: e.tensor_tensor(out=sw[6][:], in0=sw[4][:], in1=cs_, op=ALU.mult), reads=['sw4'] + TRIG, writes=['sw6'])
                    S.op('pool', lambda e: e.tensor_tensor(out=sw[7][:], in0=sw[5][:], in1=sn_, op=ALU.mult), reads=['sw5'] + TRIG, writes=['sw7'])
                    S.op('pool', lambda e: e.tensor_tensor(out=hre[:, j, :], in0=sw[6][:], in1=sw[7][:], op=ALU.subtract), reads=['sw6', 'sw7'], writes=['hre'])
                    S.op('pool', lambda e: e.tensor_tensor(out=sw[6][:], in0=sw[4][:], in1=sn_, op=ALU.mult), reads=['sw4'] + TRIG, writes=['sw6'])
                    S.op('pool', lambda e: e.tensor_tensor(out=sw[7][:], in0=sw[5][:], in1=cs_, op=ALU.mult), reads=['sw5'] + TRIG, writes=['sw7'])
                    S.op('pool', lambda e: e.tensor_tensor(out=him[:, j, :], in0=sw[6][:], in1=sw[7][:], op=ALU.add), reads=['sw6', 'sw7'], writes=['him'])
                    S.op('act', lambda e: e.activation(out=carry_re[:, j:j + 1], in_=hre[:, j, 255:256], func=AF.Copy), reads=['hre'], writes=['carry'])
                    S.op('act', lambda e: e.activation(out=carry_im[:, j:j + 1], in_=him[:, j, 255:256], func=AF.Copy), reads=['him'], writes=['carry'])
                py = pA[nxt('pA')]; pyn = f"pA{rot['pA'] ^ 1}"
                for j in range(4):
                    mm(py[:, 0:256], CCre[:, j, :], hre[:, j, :], j == 0, False, ['CC', 'hre'], [pyn])
                    mm(py[:, 0:256], CCim[:, j, :], him[:, j, :], False, j == 3, ['CC', 'him'], [pyn])
                S.op('dve', lambda e: e.scalar_tensor_tensor(out=yt[:, c0:c0 + 256], in0=uT[:, c0:c0 + 256], scalar=dcol[:, 0:1], in1=py[:, 0:256],
                                                             op0=ALU.mult, op1=ALU.add), reads=['uT', 'dcol', pyn], writes=[yn])
            S.op('act', lambda e: e.activation(out=yt[:], in_=yt[:], func=AF.Gelu_apprx_tanh), reads=[yn], writes=[yn])
            S.dma_start('sp', o_ssm[:, t0:t0 + 512], yt[:], reads=[yn], is_output=True)

            for ts in range(4):
                i = I * 4 + ts
                Qi = qT[:, ts, :, :].rearrange("p a b -> p (a b)")

                def branch(br, blocks):
                    oi = nxt('pO'); po = pO[oi]; pon = f"pO{oi}"
                    S.op('act', lambda e: e.activation(out=po[:], in_=zer[:, 0:260].rearrange("p (a b) -> p a b", a=4), func=AF.Copy), reads=['zer'], writes=[pon])
                    if br == 0:
                        S.op('act', lambda e: e.activation(out=pI[:], in_=zer[:, :].rearrange("p (a b) -> p a b", a=4), func=AF.Copy), reads=['zer'], writes=['pI', 'pM0', 'pM1'])

                    def stage1(blk):
                        (kl, kreads, masks, vr, vreads, ovr) = blk[:6]
                        masks = [m_() if callable(m_) else m_ for m_ in masks]
                        si = nxt('pS', 2); psn = f"pS{si}"; p = pS[si]
                        mm(p[:], kl, Qi, True, len(masks) == 0, kreads + ['qT'], [psn])
                        for mi, (ml, mreads) in enumerate(masks):
                            mm(p[:], ml, id4[:], False, mi == len(masks) - 1, mreads + ['id4'], [psn])
                        mulm = blk[6]() if len(blk) > 6 else None
                        return p, psn, mulm

                    def stage2(blk, p, psn, mulm):
                        (kl, kreads, masks, vr, vreads, ovr) = blk[:6]
                        pi_ = nxt('pT'); ptile = pT[pi_]; ptn = f"pT{pi_}"
                        S.op('act', lambda e: e.activation(out=ptile[:], in_=p[:], func=AF.Exp, scale=0.125), reads=[psn], writes=[ptn])
                        if mulm is not None:
                            pm, pmn = mulm
                            S.op('dve', lambda e: e.tensor_tensor(out=ptile[:].rearrange("p (a b) -> p a b", a=4), in0=ptile[:].rearrange("p (a b) -> p a b", a=4),
                                                                  in1=pm.unsqueeze(1).to_broadcast([128, 4, 128]), op=ALU.mult), reads=[ptn, pmn], writes=[ptn])
                        for h in range(4):
                            mm(po[:, h, :], ptile[:, h * 128:(h + 1) * 128], vr, False, False, [ptn] + vreads, [pon], skip=True)
                            if ovr is not None:
                                mm(pI[:, h, :], ptile[:, h * 128:(h + 1) * 128], ovr, False, False, [ptn, 'ov'], ['pI'], skip=True)

                    prev = None
                    for blk in blocks:
                        cur = stage1(blk)
                        if prev is not None:
                            stage2(*prev)
                        prev = (blk,) + cur
                    stage2(*prev)
                    S.op('act', lambda e: e.activation(out=osb[br][:], in_=po[:], func=AF.Copy), reads=[pon], writes=[f'osb{br}'])
                    S.op('dve', lambda e: e.tensor_scalar(out=den[:, br, :], in0=osb[br][:, :, 64], scalar1=1e-30, scalar2=None, op0=ALU.max),
                         reads=[f'osb{br}'], writes=['den'])
                    S.op('dve', lambda e: e.reciprocal(out=den[:, br, :], in_=den[:, br, :]), reads=['den'], writes=['den'])

                nb = (8 * i + 7) // 128 + 1
                blocks = []
                for jb in range(nb):
                    masks = []
                    if (i, jb) in _CMKEY:
                        def mc_(jb=jb):
                            ci = nxt('cmt'); ct = cmt[ci]; cn = f"cmt{ci}"
                            S.dma_start('sp', ct[:], c_cm[_CMKEY[(i, jb)]], writes=[cn])
                            return (ct[:], [cn])
                        masks.append(mc_)
                    blocks.append((kcT[:, jb * 128:(jb + 1) * 128], ['kcT'], masks, vc[:, jb, :], ['vc'], ov[:, jb, :]))
                branch(0, blocks)
                S.op('dve', lambda e: e.tensor_scalar(out=imp[:], in0=pI[:, 0, :], scalar1=den[:, 0, 0:1], scalar2=None, op0=ALU.mult), reads=['pI', 'den'], writes=['imp'])
                for h in range(1, 4):
                    S.op('dve', lambda e: e.scalar_tensor_tensor(out=imp[:], in0=pI[:, h, :], scalar=den[:, 0, h:h + 1], in1=imp[:], op0=ALU.mult, op1=ALU.add),
                         reads=['pI', 'den', 'imp'], writes=['imp'])
                off = 128 - 2 * i
                S.op('dve', lambda e: e.tensor_tensor(out=imp2[:], in0=imp[:], in1=keep[:, off:off + 128], op=ALU.mult), reads=['imp', 'keep'], writes=['imp2'])
                S.op('dve', lambda e: e.tensor_tensor(out=imp2[:], in0=imp2[:], in1=addt[:, off:off + 128], op=ALU.add), reads=['imp2', 'add'], writes=['imp2'])
                S.op('dve', lambda e: e.memset(imp2[:, 0:1], 3.0e4), writes=['imp2'])
                S.op('dve', lambda e: e.max(out=m8[:], in_=imp2[:]), reads=['imp2'], writes=['m8'])
                S.op('dve', lambda e: e.match_replace(out=wk[:], in_to_replace=m8[:], in_values=imp2[:], imm_value=-5.0), reads=['imp2', 'm8'], writes=['wk'])
                S.op('dve', lambda e: e.max(out=m8b[:], in_=wk[:]), reads=['wk'], writes=['m8b'])
                S.op('dve', lambda e: e.tensor_scalar(out=negm[:], in0=imp2[:], scalar1=m8b[:, 7:8], scalar2=None, op0=ALU.is_ge),
                     reads=['imp2', 'm8b'], writes=['negm'])
                if debug:
                    S.dma_start('sp', o_dbg[i, :, 0, :], imp[:], reads=['imp'], is_output=True)
                    S.dma_start('sp', o_dbg[i, :, 1, :], imp2[:], reads=['imp2'], is_output=True)
                    S.dma_start('sp', o_dbg[i, :, 2, :], negm[:], reads=['negm'], is_output=True)
                    S.dma_start('sp', o_dbg[i, :, 3, :], wk[:], reads=['wk'], is_output=True)
                blocks = []
                for j in range(max(0, i - 4), i + 1):
                    masks = []
                    if j == i:
                        masks.append((triq[:], ['triq']))
                    if j == i - 4:
                        masks.append((tri2q[:], ['tri2q']))
                    s8 = j % 8
                    blocks.append((kwr[:, s8 * 128:(s8 + 1) * 128], [f'kwr{s8 // 4}'], masks, vwr[:, s8, :], [f'vwr{s8}'], None))
                branch(2, blocks)
                blocks = []
                for j in range(i + 1):
                    def mk_(j=j):
                        ni = nxt('ne'); ne = negexp[ni]; nn = f"negexp{ni}"
                        S.op('pool', lambda e: e.tensor_copy(out=ne[:].rearrange("p (a b) -> p a b", a=2), in_=negm[:, 2 * j:2 * j + 2].unsqueeze(2).to_broadcast([128, 2, 64])),
                             reads=['negm'], writes=[nn])
                        mi_ = nxt('pM', 2); pmn = f"pM{mi_}"
                        pm = pI[:, 0, :] if mi_ == 0 else pMb[:, 0:128]
                        S.op('pe', lambda e: e.transpose(out=pm, in_=ne[:], identity=ident[:]), reads=[nn, 'ident'], writes=[pmn] + (['pI'] if mi_ == 0 else []))
                        return (pm, pmn)
                    masks = []
                    if j == i:
                        masks.append((triq[:], ['triq']))
                    blocks.append((KT[:, j * 128:(j + 1) * 128], [f'KT{j // 4}'], masks, vslc[:, j, :], [f'vslc{j}'], None, mk_))
                branch(1, blocks)
                S.op('dve', lambda e: e.tensor_tensor(out=coef[:].rearrange("p a b -> p (a b)"), in0=den[:].rearrange("p a b -> p (a b)"), in1=gts[:, ts, :], op=ALU.mult),
                     reads=['den', 'gts'], writes=['coef'])
                ai = nxt('aout'); ao = aout[ai]; an_ = f"aout{ai}"
                for h in range(4):
                    S.op('dve', lambda e: e.tensor_scalar(out=ao[:, h * 64:(h + 1) * 64], in0=osb[0][:, h, 0:64], scalar1=coef[:, 0, h:h + 1], scalar2=None, op0=ALU.mult),
                         reads=['osb0', 'coef'], writes=[an_])
                    for br in (1, 2):
                        S.op('dve', lambda e: e.scalar_tensor_tensor(out=ao[:, h * 64:(h + 1) * 64], in0=osb[br][:, h, 0:64], scalar=coef[:, br, h:h + 1],
                                                                     in1=ao[:, h * 64:(h + 1) * 64], op0=ALU.mult, op1=ALU.add),
                             reads=[f'osb{br}', 'coef', an_], writes=[an_])
                S.dma_start('sp', o_attn[i * 128:(i + 1) * 128, :], ao[:], reads=[an_], is_output=True)
        S.finish('sp')
    return nc, S


def _barrier(S):
    toks = []
    for k in S.eng:
        if S.cnt[k] > 0:
            toks.append((S.sem[k][0], S.sem[k][1], S.cnt[k], k))
    for h, val, sid in S.dma:
        if val > 0:
            toks.append((h, sid, val, 'dma'))
    for e in S.eng:
        for t in toks:
            S._wait(e, t)


NTOK = 2048


def build_B():
    nc = bass.Bass("TRN2", target_bir_lowering=False)

    def din(name, shape):
        return nc.dram_tensor(name, list(shape), F32, kind="ExternalInput").ap()

    xT = din("xT", [8, 128, NTOK]); aT = din("aT", [8, 128, NTOK]); sT = din("sT", [4, 128, NTOK])
    cst = din("cst", [128, 8])
    adaw = din("adaw", [48, 128, 8, 128]); adab = din("adab", [128, 48])
    n1g = din("n1g", [128, 8]); n2g = din("n2g", [128, 8]); fing = din("fing", [128, 8])
    wmg = din("wmg", [16, 128, 8, 128])
    wglu = din("wglu", [4, 128, 4, 128])
    wnso = din("wnso", [8, 128, 8, 128])
    wsso = din("wsso", [8, 128, 4, 128])
    wout = din("wout", [8, 128, 8, 128])
    wr = din("wr", [128, 8, 36]); br = din("br", [1, 36])
    mwg = din("mwg", [32, 1024, 256]); mwu = din("mwu", [32, 1024, 256]); mwd = din("mwd", [32, 256, 1024])
    c_ident = din("ident", [128, 128]); c_R = din("Rsel", [32, 4096])
    o_x = nc.dram_tensor("o_x", [8, 128, NTOK], F32, kind="ExternalOutput").ap()
    o_n = nc.dram_tensor("o_n", [8, 128, NTOK], F32, kind="ExternalOutput").ap()

    S = Sch(nc)
    with contextlib.ExitStack() as st:
        def sb(name, shape, dt=F32):
            return st.enter_context(nc.sbuf_tensor(name, list(shape), dt))

        def ps(name, shape):
            return st.enter_context(nc.psum_tensor(name, list(shape), F32))

        ones = sb("ones", [128, 128]); ident = sb("ident_s", [128, 128]); Rsel = sb("Rsel_s", [32, 4096])
        modc = sb("modc", [128, 48]); scl1 = sb("scl1", [128, 8]); scl2 = sb("scl2", [128, 8]); fg = sb("fg", [128, 8])
        biasmg = sb("biasmg", [1, 2048]); wrs = sb("wrs", [128, 8, 36]); brs = sb("brs", [1, 36])
        xt = sb("xt", [128, 8, 512]); x1 = sb("x1", [128, 8, 512]); h2 = sb("h2", [128, 8, 512])
        xsq = [sb(f"xsq{i}", [128, 512]) for i in range(2)]
        sqbc = sb("sqbc", [128, 512]); rbc = sb("rbc", [128, 512])
        wgtT = sb("wgtT", [32, 512])
        lg = sb("lg", [128, 36]); gmax = sb("gmax", [128, 1]); ngmax = sb("ngmax", [128, 1]); eg = sb("eg", [128, 4]); gsum = sb("gsum", [128, 1])
        oh = sb("oh", [128, 4]); msk = sb("msk", [128, 32]); m8 = sb("m8", [128, 8]); nl1 = sb("nl1", [128, 1]); ew = sb("ew", [128, 32])
        sel2 = sb("sel2", [128, 32]); s2 = sb("s2", [128, 1]); wgt = sb("wgt", [128, 32])

        pA = [ps(f"pA{i}", [128, 512]) for i in range(2)]
        pG = [ps(f"pG{i}", [128, 512]) for i in range(2)]
        pU = [ps(f"pU{i}", [128, 512]) for i in range(2)]
        pD = [ps(f"pD{i}", [128, 512]) for i in range(2)]
        rot = {}

        def nxt(k, n=2):
            v = rot.get(k, 0); rot[k] = (v + 1) % n
            return v

        def mm(out, lhsT, rhs, start, stop, reads, writes):
            S.op('pe', lambda e: e.matmul(out, lhsT=lhsT, rhs=rhs, start=start, stop=stop), reads=reads, writes=writes)

        def pget(pool, name):
            i = nxt(name)
            return pool[i], f"{name}{i}"

        S.dma_start('sp', ident[:], c_ident, writes=['ident'])
        S.dma_start('sp', Rsel[:], c_R, writes=['Rsel'])
        S.dma_start('sp', wrs[:], wr, writes=['wrs'])
        S.dma_start('sp', brs[:], br, writes=['brs'])
        S.dma_start('sp', fg[:], fing, writes=['fg'])
        S.op('pool', lambda e: e.memset(ones[:], 1.0), writes=['ones'])

        with contextlib.ExitStack() as st2:
            def sb2(name, shape, dt=F32):
                return st2.enter_context(nc.sbuf_tensor(name, list(shape), dt))
            cs = sb2("cs", [128, 8]); sg = sb2("sg", [128, 8])
            slab = [sb2(f"pslab{i}", [128, 8, 128]) for i in range(2)]
            adabs = sb2("adabs", [128, 48]); n1 = sb2("n1", [128, 8]); n2 = sb2("n2", [128, 8])
            S.dma_start('sp', cs[:], cst, writes=['cs'])
            S.dma_start('sp', adabs[:], adab, writes=['adabs'])
            S.dma_start('sp', n1[:], n1g, writes=['n1']); S.dma_start('sp', n2[:], n2g, writes=['n2'])
            S.op('act', lambda e: e.activation(out=sg[:], in_=cs[:], func=AF.Sigmoid), reads=['cs'], writes=['sg'])
            S.op('dve', lambda e: e.tensor_tensor(out=cs[:], in0=cs[:], in1=sg[:], op=ALU.mult), reads=['cs', 'sg'], writes=['cs'])
            for j in range(48):
                a = slab[j % 2]; an = f"pslab{j % 2}"
                S.dma_start('sp', a[:], adaw[j], writes=[an])
                p, pn = pget(pA, 'pA')
                for kc in range(8):
                    mm(p[:, 0:1], a[:, kc, :], cs[:, kc:kc + 1], kc == 0, kc == 7, [an, 'cs'], [pn])
                S.op('dve', lambda e: e.tensor_tensor(out=modc[:, j:j + 1], in0=p[:, 0:1], in1=adabs[:, j:j + 1], op=ALU.add),
                     reads=[pn, 'adabs'], writes=['modc'])
            for c in range(16):
                a = slab[c % 2]; an = f"pslab{c % 2}"
                S.dma_start('sp', a[:], wmg[c], writes=[an])
                p, pn = pget(pA, 'pA')
                for kc in range(8):
                    mm(p[0:1, 0:128], modc[:, kc:kc + 1], a[:, kc, :], kc == 0, kc == 7, ['modc', an], [pn])
                S.op('act', lambda e: e.activation(out=biasmg[0:1, c * 128:(c + 1) * 128], in_=p[0:1, 0:128], func=AF.Copy), reads=[pn], writes=['biasmg'])
            S.op('dve', lambda e: e.tensor_scalar(out=scl1[:], in0=modc[:, 8:16], scalar1=1.0, scalar2=None, op0=ALU.add), reads=['modc'], writes=['scl1'])
            S.op('dve', lambda e: e.tensor_tensor(out=scl1[:], in0=scl1[:], in1=n1[:], op=ALU.mult), reads=['scl1', 'n1'], writes=['scl1'])
            S.op('dve', lambda e: e.tensor_scalar(out=scl2[:], in0=modc[:, 32:40], scalar1=1.0, scalar2=None, op0=ALU.add), reads=['modc'], writes=['scl2'])
            S.op('dve', lambda e: e.tensor_tensor(out=scl2[:], in0=scl2[:], in1=n2[:], op=ALU.mult), reads=['scl2', 'n2'], writes=['scl2'])
        _barrier(S)

        def rms_stats(src, srcname):
            p, pn = pget(pA, 'pA')
            for kc in range(8):
                xi = nxt('xsq'); xs = xsq[xi]; xn = f"xsq{xi}"
                S.op('act', lambda e: e.activation(out=xs[:], in_=src[:, kc, :], func=AF.Square), reads=[srcname], writes=[xn])
                mm(p[:], ones[:], xs[:], kc == 0, kc == 7, ['ones', xn], [pn])
            S.op('act', lambda e: e.activation(out=sqbc[:], in_=p[:], func=AF.Sqrt, scale=1.0 / D, bias=EPS), reads=[pn], writes=['sqbc'])
            S.op('dve', lambda e: e.reciprocal(out=rbc[:], in_=sqbc[:]), reads=['sqbc'], writes=['rbc'])

        for T in range(NTOK // 512):
            t0 = T * 512
            with contextlib.ExitStack() as st3:
                def sb3(name, shape, dt=F32):
                    return st3.enter_context(nc.sbuf_tensor(f"{name}_t{T}", list(shape), dt))
                at = sb3("at", [128, 8, 512]); stt = sb3("stt", [128, 4, 512]); hs = sb3("hs", [128, 8, 512])
                ssmg = sb3("ssmg", [128, 4, 512]); merged = sb3("merged", [128, 8, 512])
                slabs = [sb3(f"slab{i}", [128, 8, 128]) for i in range(4)]
                ga = sb3("ga", [128, 512]); gs = sb3("gs", [128, 512]); m2 = sb3("m2", [128, 512]); tmp = sb3("tmp", [128, 512])

                def get_slab(src, kcn):
                    i = nxt('slab', 4)
                    S.dma_start('sp', slabs[i][:, 0:kcn, :], src, writes=[f"slab{i}"])
                    return slabs[i], f"slab{i}"

                S.dma_start('sp', xt[:], xT[:, :, t0:t0 + 512].rearrange("k p t -> p k t"), writes=['xt'])
                S.dma_start('sp', at[:], aT[:, :, t0:t0 + 512].rearrange("k p t -> p k t"), writes=['at'])
                S.dma_start('sp', stt[:], sT[:, :, t0:t0 + 512].rearrange("k p t -> p k t"), writes=['stt'])
                rms_stats(xt, 'xt')
                for kc in range(8):
                    S.op('pool', lambda e: e.tensor_scalar(out=hs[:, kc, :], in0=xt[:, kc, :], scalar1=scl1[:, kc:kc + 1], scalar2=None, op0=ALU.mult),
                         reads=['xt', 'scl1'], writes=['hs'])
                for c in range(4):
                    sl, sn = get_slab(wglu[c], 4)
                    p, pn = pget(pA, 'pA')
                    for kc in range(4):
                        mm(p[:], sl[:, kc, :], stt[:, kc, :], kc == 0, kc == 3, [sn, 'stt'], [pn])
                    S.op('act', lambda e: e.activation(out=tmp[:], in_=p[:], func=AF.Sigmoid), reads=[pn], writes=['tmp'])
                    S.op('dve', lambda e: e.tensor_tensor(out=ssmg[:, c, :], in0=stt[:, c, :], in1=tmp[:], op=ALU.mult), reads=['stt', 'tmp'], writes=['ssmg'])
                for c in range(8):
                    for (gi, gdst, gname) in ((c, ga, 'ga'), (8 + c, gs, 'gs')):
                        sl, sn = get_slab(wmg[gi], 8)
                        p, pn = pget(pA, 'pA')
                        for kc in range(8):
                            mm(p[:], sl[:, kc, :], hs[:, kc, :], kc == 0, False, [sn, 'hs'], [pn])
                        mm(p[:], biasmg[0:1, gi * 128:(gi + 1) * 128], sqbc[0:1, :], False, True, ['biasmg', 'sqbc'], [pn])
                        S.op('dve', lambda e: e.tensor_tensor(out=gdst[:], in0=p[:], in1=rbc[:], op=ALU.mult), reads=[pn, 'rbc'], writes=[gname])
                        S.op('act', lambda e: e.activation(out=gdst[:], in_=gdst[:], func=AF.Sigmoid), reads=[gname], writes=[gname])
                    sl, sn = get_slab(wnso[c], 8)
                    p, pn = pget(pA, 'pA')
                    for kc in range(8):
                        mm(p[:], sl[:, kc, :], at[:, kc, :], kc == 0, kc == 7, [sn, 'at'], [pn])
                    S.op('dve', lambda e: e.tensor_tensor(out=merged[:, c, :], in0=p[:], in1=ga[:], op=ALU.mult), reads=[pn, 'ga'], writes=['merged'])
                    sl, sn = get_slab(wsso[c], 4)
                    p, pn = pget(pA, 'pA')
                    for kc in range(4):
                        mm(p[:], sl[:, kc, :], ssmg[:, kc, :], kc == 0, kc == 3, [sn, 'ssmg'], [pn])
                    S.op('dve', lambda e: e.tensor_tensor(out=m2[:], in0=p[:], in1=gs[:], op=ALU.mult), reads=[pn, 'gs'], writes=['m2'])
                    S.op('pool', lambda e: e.tensor_tensor(out=merged[:, c, :], in0=merged[:, c, :], in1=m2[:], op=ALU.add), reads=['merged', 'm2'], writes=['merged'])
                for c in range(8):
                    sl, sn = get_slab(wout[c], 8)
                    p, pn = pget(pA, 'pA')
                    for kc in range(8):
                        mm(p[:], sl[:, kc, :], merged[:, kc, :], kc == 0, kc == 7, [sn, 'merged'], [pn])
                    S.op('dve', lambda e: e.scalar_tensor_tensor(out=x1[:, c, :], in0=p[:], scalar=modc[:, 16 + c:17 + c], in1=xt[:, c, :], op0=ALU.mult, op1=ALU.add),
                         reads=[pn, 'modc', 'xt'], writes=['x1'])
                rms_stats(x1, 'x1')
                for kc in range(8):
                    S.op('dve', lambda e: e.tensor_tensor(out=h2[:, kc, :], in0=x1[:, kc, :], in1=rbc[:], op=ALU.mult), reads=['x1', 'rbc'], writes=['h2'])
                    S.op('pool', lambda e: e.tensor_scalar(out=h2[:, kc, :], in0=h2[:, kc, :], scalar1=scl2[:, kc:kc + 1], scalar2=modc[:, 24 + kc:25 + kc], op0=ALU.mult, op1=ALU.add),
                         reads=['h2', 'scl2', 'modc'], writes=['h2'])
                for ts in range(4):
                    p, pn = pget(pA, 'pA')
                    for kc in range(8):
                        mm(p[:, 0:36], h2[:, kc, ts * 128:(ts + 1) * 128], wrs[:, kc, :], kc == 0, False, ['h2', 'wrs'], [pn])
                    mm(p[:, 0:36], ones[0:1, 0:128], brs[0:1, :], False, True, ['ones', 'brs'], [pn])
                    S.op('act', lambda e: e.activation(out=lg[:], in_=p[:, 0:36], func=AF.Copy), reads=[pn], writes=['lg'])
                    dv = lambda fn, r, w: S.op('dve', fn, reads=r, writes=w)
                    dv(lambda e: e.tensor_reduce(out=gmax[:], in_=lg[:, 0:4], axis=AX.X, op=ALU.max), ['lg'], ['gmax'])
                    dv(lambda e: e.tensor_scalar(out=ngmax[:], in0=gmax[:], scalar1=-1.0, scalar2=None, op0=ALU.mult), ['gmax'], ['ngmax'])
                    S.op('act', lambda e: e.activation(out=eg[:], in_=lg[:, 0:4], func=AF.Exp, bias=ngmax[:, 0:1]), reads=['lg', 'ngmax'], writes=['eg'])
                    dv(lambda e: e.tensor_reduce(out=gsum[:], in_=eg[:], axis=AX.X, op=ALU.add), ['eg'], ['gsum'])
                    dv(lambda e: e.reciprocal(out=gsum[:], in_=gsum[:]), ['gsum'], ['gsum'])
                    dv(lambda e: e.tensor_scalar(out=oh[:], in0=lg[:, 0:4], scalar1=gmax[:, 0:1], scalar2=None, op0=ALU.is_ge), ['lg', 'gmax'], ['oh'])
                    dv(lambda e: e.tensor_scalar(out=oh[:], in0=oh[:], scalar1=1.0, scalar2=1.0e9, op0=ALU.subtract, op1=ALU.mult), ['oh'], ['oh'])
                    for g in range(4):
                        dv(lambda e: e.tensor_scalar(out=msk[:, g * 8:(g + 1) * 8], in0=lg[:, 4 + g * 8:12 + g * 8], scalar1=oh[:, g:g + 1], scalar2=None, op0=ALU.add),
                           ['lg', 'oh'], ['msk'])
                    dv(lambda e: e.max(out=m8[:], in_=msk[:]), ['msk'], ['m8'])
                    dv(lambda e: e.tensor_scalar(out=nl1[:], in0=m8[:, 0:1], scalar1=-1.0, scalar2=None, op0=ALU.mult), ['m8'], ['nl1'])
                    S.op('act', lambda e: e.activation(out=ew[:], in_=msk[:], func=AF.Exp, bias=nl1[:, 0:1]), reads=['msk', 'nl1'], writes=['ew'])
                    dv(lambda e: e.tensor_scalar(out=sel2[:], in0=msk[:], scalar1=m8[:, 1:2], scalar2=None, op0=ALU.is_ge), ['msk', 'm8'], ['sel2'])
                    dv(lambda e: e.tensor_tensor(out=ew[:], in0=ew[:], in1=sel2[:], op=ALU.mult), ['ew', 'sel2'], ['ew'])
                    dv(lambda e: e.tensor_reduce(out=s2[:], in_=ew[:], axis=AX.X, op=ALU.add), ['ew'], ['s2'])
                    dv(lambda e: e.reciprocal(out=s2[:], in_=s2[:]), ['s2'], ['s2'])
                    dv(lambda e: e.tensor_tensor(out=s2[:], in0=s2[:], in1=gsum[:], op=ALU.mult), ['s2', 'gsum'], ['s2'])
                    dv(lambda e: e.tensor_scalar(out=wgt[:], in0=ew[:], scalar1=s2[:, 0:1], scalar2=None, op0=ALU.mult), ['ew', 's2'], ['wgt'])
                    p2, pn2 = pget(pA, 'pA')
                    S.op('pe', lambda e: e.transpose(out=p2[0:32, 0:128], in_=wgt[:], identity=ident[:]), reads=['wgt', 'ident'], writes=[pn2])
                    S.op('act', lambda e: e.activation(out=wgtT[:, ts * 128:(ts + 1) * 128], in_=p2[0:32, 0:128], func=AF.Copy), reads=[pn2], writes=['wgtT'])
            _barrier(S)
            with contextlib.ExitStack() as st4:
                def sb4(name, shape, dt=F32):
                    return st4.enter_context(nc.sbuf_tensor(f"{name}_t{T}", list(shape), dt))
                acc = sb4("acc", [128, 8, 512])
                wg = [sb4(f"wg{i}", [128, 8, 256]) for i in range(2)]
                wu = [sb4(f"wu{i}", [128, 8, 256]) for i in range(2)]
                wd = [sb4(f"wd{i}", [128, 2, 1024]) for i in range(2)]
                hid = [sb4(f"hid{i}", [128, 512]) for i in range(4)]
                actb = [sb4(f"actb{i}", [128, 512]) for i in range(2)]
                wbc = [sb4(f"wbc{i}", [128, 512]) for i in range(2)]
                S.op('pool', lambda e: e.memset(acc[:], 0.0), writes=[f'acc{c}' for c in range(8)])
                def down(ex):
                    b = ex % 2
                    for c in range(8):
                        pd, pdn = pget(pD, 'pD')
                        for hc in range(2):
                            mm(pd[:], wd[b][:, hc, c * 128:(c + 1) * 128], hid[b * 2 + hc][:], hc == 0, hc == 1, [f'wd{b}', f'hid{b * 2 + hc}'], [pdn])
                        S.op('dve', lambda e: e.tensor_tensor(out=acc[:, c, :], in0=pd[:], in1=acc[:, c, :], op=ALU.add), reads=[pdn, f'acc{c}'], writes=[f'acc{c}'])

                for ex in range(32):
                    b = ex % 2
                    S.dma_start('sp', wg[b][:], mwg[ex].rearrange("(k p) n -> p k n", p=128), writes=[f'wg{b}'])
                    S.dma_start('sp', wu[b][:], mwu[ex].rearrange("(k p) n -> p k n", p=128), writes=[f'wu{b}'])
                    S.dma_start('sp', wd[b][:], mwd[ex].rearrange("(k p) n -> p k n", p=128), writes=[f'wd{b}'])
                    p, pn = pget(pA, 'pA')
                    mm(p[:], Rsel[:, ex * 128:(ex + 1) * 128], wgtT[:], True, True, ['Rsel', 'wgtT'], [pn])
                    S.op('act', lambda e: e.activation(out=wbc[b][:], in_=p[:], func=AF.Copy), reads=[pn], writes=[f'wbc{b}'])
                    for hc in range(2):
                        hi = b * 2 + hc
                        pg, pgn = pget(pG, 'pG')
                        pu, pun = pget(pU, 'pU')
                        for kc in range(8):
                            mm(pg[:], wg[b][:, kc, hc * 128:(hc + 1) * 128], h2[:, kc, :], kc == 0, kc == 7, [f'wg{b}', 'h2'], [pgn])
                        for kc in range(8):
                            mm(pu[:], wu[b][:, kc, hc * 128:(hc + 1) * 128], h2[:, kc, :], kc == 0, kc == 7, [f'wu{b}', 'h2'], [pun])
                        S.op('act', lambda e: e.activation(out=actb[hc][:], in_=pg[:], func=AF.Silu), reads=[pgn], writes=[f'actb{hc}'])
                        S.op('dve', lambda e: e.tensor_tensor(out=hid[hi][:], in0=pu[:], in1=actb[hc][:], op=ALU.mult), reads=[pun, f'actb{hc}'], writes=[f'hid{hi}'])
                        S.op('pool', lambda e: e.tensor_tensor(out=hid[hi][:], in0=hid[hi][:], in1=wbc[b][:], op=ALU.mult), reads=[f'hid{hi}', f'wbc{b}'], writes=[f'hid{hi}'])
                    if ex > 0:
                        down(ex - 1)
                down(31)
                for c in range(8):
                    S.op('dve', lambda e: e.scalar_tensor_tensor(out=acc[:, c, :], in0=acc[:, c, :], scalar=modc[:, 40 + c:41 + c], in1=x1[:, c, :], op0=ALU.mult, op1=ALU.add),
                         reads=[f'acc{c}', 'modc', 'x1'], writes=[f'acc{c}', 'accall'])
                S.dma_start('sp', o_x[:, :, t0:t0 + 512].rearrange("k p t -> p k t"), acc[:], reads=['accall'] + [f'acc{c}' for c in range(8)], is_output=True)
                rms_stats(acc, 'accall')
                for c in range(8):
                    S.op('dve', lambda e: e.tensor_tensor(out=h2[:, c, :], in0=acc[:, c, :], in1=rbc[:], op=ALU.mult), reads=['accall', 'rbc'], writes=['h2'])
                    S.op('pool', lambda e: e.tensor_scalar(out=h2[:, c, :], in0=h2[:, c, :], scalar1=fg[:, c:c + 1], scalar2=None, op0=ALU.mult), reads=['h2', 'fg'], writes=['h2'])
                S.dma_start('sp', o_n[:, :, t0:t0 + 512].rearrange("k p t -> p k t"), h2[:], reads=['h2'], is_output=True)
            _barrier(S)
        S.finish('sp')
    return nc, S


def _chunkT(a):
    T, Fd = a.shape
    return np.ascontiguousarray(a.T.reshape(Fd // 128, 128, T))


def _col(v):
    return np.ascontiguousarray(v.reshape(-1, 128).T)


def _slab(w):
    K, N = w.shape
    return np.ascontiguousarray(w.reshape(K // 128, 128, N // 128, 128).transpose(2, 1, 0, 3))


def _pkn(w):
    K, N = w.shape
    return np.ascontiguousarray(w.reshape(K // 128, 128, N).transpose(1, 0, 2))


_PROG = {}


def _get_prog(name):
    if name not in _PROG:
        _PROG[name] = build_A()[0] if name == 'A' else build_B()[0]
    return _PROG[name]


def _a_inputs(l, b, g, xTb, I):
    f = np.float32
    d = {}
    d['xT'] = xTb
    d['cst'] = _col(I['c'][b])
    d['adaw'] = _slab(I['ada_w'][l][:, :2048])
    d['adab'] = _col(I['ada_b'][l][:2048])
    d['n1g'] = _col(I['norm1_g'][l])
    d['wsel'] = _pkn(I['w_in'][l][:, _sel_cols(g)])
    w1 = I['nsa_cmp_w1'][l]
    d['w1d'] = np.ascontiguousarray(
        np.stack([w1[kv].reshape(16, 2, 64, 2, 128).transpose(3, 1, 2, 0, 4).reshape(2, 128, 16, 128) for kv in range(2)]).reshape(4, 128, 16, 128))
    d['pos2'] = np.ascontiguousarray(I['nsa_cmp_pos'][l].reshape(2, 16, 2, 64).transpose(2, 3, 0, 1).reshape(128, 2, 16))
    d['w2d'] = np.ascontiguousarray(I['nsa_cmp_w2'][l].reshape(2, 2, 128, 64).transpose(2, 0, 1, 3))
    for k in ('triq', 'tri2q', 'ident', 'id4', 'keep', 'add', 'ov', 'cm'):
        d[k] = _CONST[k]
    G0 = 8 * g
    lr = I['ssm_lam_re'][l][G0:G0 + 8]; li = I['ssm_lam_im'][l][G0:G0 + 8]; ldt = I['ssm_log_dt'][l][G0:G0 + 8]
    ldt_full = np.repeat(ldt[:, None], 64, axis=1)
    d['s_lr'] = np.ascontiguousarray(np.tile(lr.reshape(1, 512), (128, 1)))
    d['s_li'] = np.ascontiguousarray(np.tile(li.reshape(1, 512), (128, 1)))
    d['s_ldt'] = np.ascontiguousarray(np.tile(ldt_full.reshape(1, 512), (128, 1)))
    zb_re = np.zeros((8, 16, 8, 64), f); zb_im = np.zeros((8, 16, 8, 64), f)
    zc_re = np.zeros((2, 64, 4, 8, 16), f); zc_im = np.zeros((2, 64, 4, 8, 16), f)
    for gl in range(8):
        zb_re[gl, :, gl, :] = I['ssm_b_re'][l][G0 + gl].T
        zb_im[gl, :, gl, :] = I['ssm_b_im'][l][G0 + gl].T
        j, gl2 = gl // 2, gl % 2
        zc_re[gl2, :, j, gl, :] = I['ssm_c_re'][l][G0 + gl].T
        zc_im[gl2, :, j, gl, :] = I['ssm_c_im'][l][G0 + gl].T
    d['s_bre'] = zb_re.reshape(128, 512); d['s_bim'] = zb_im.reshape(128, 512)
    d['s_cre'] = zc_re.reshape(128, 4, 128); d['s_cim'] = zc_im.reshape(128, 4, 128)
    d['s_lrc'] = np.ascontiguousarray(lr.reshape(4, 2, 64).transpose(1, 2, 0).reshape(128, 4))
    d['s_lic'] = np.ascontiguousarray(li.reshape(4, 2, 64).transpose(1, 2, 0).reshape(128, 4))
    d['s_ldtc'] = np.ascontiguousarray(ldt_full.reshape(4, 2, 64).transpose(1, 2, 0).reshape(128, 4))
    d['s_d'] = np.ascontiguousarray(I['ssm_d'][l][G0:G0 + 8].reshape(128, 1))
    d['s_iota'] = np.ascontiguousarray(np.tile(np.arange(512, dtype=f)[None], (128, 1)))
    return {k: np.ascontiguousarray(v, dtype=f) for k, v in d.items()}


def _run_A(l, xT_all, I):
    nc = _get_prog('A')
    maps = []
    for core in range(8):
        b, g = core // 4, core % 4
        maps.append(_a_inputs(l, b, g, xT_all[b], I))
    res = run_bass_kernel_spmd(nc, maps, core_ids=list(range(8))).results
    attn = np.zeros((2, SEQ, 1024), np.float32)
    ssmT = np.zeros((2, 512, SEQ), np.float32)
    for core in range(8):
        b, g = core // 4, core % 4
        attn[b][:, g * 256:(g + 1) * 256] = res[core]['o_attn']
        ssmT[b][g * 128:(g + 1) * 128, :] = res[core]['o_ssm']
    return attn, ssmT


def _run_B(l, xT_all, attn, ssmT, I):
    nc = _get_prog('B')
    f = np.float32
    o4 = 1024 + 1536 + 48 + 512
    common = dict(
        adaw=_slab(I['ada_w'][l]), adab=_col(I['ada_b'][l]), n1g=_col(I['norm1_g'][l]), n2g=_col(I['norm2_g'][l]),
        fing=_col(I['final_g']), wmg=_slab(I['w_in'][l][:, o4:o4 + 2048]), wglu=_slab(I['ssm_w_glu'][l]),
        wnso=_slab(I['nsa_w_o'][l]), wsso=_slab(I['ssm_w_o'][l]), wout=_slab(I['w_out'][l]),
        wr=_pkn(np.concatenate([I['moe_w_group'][l], I['moe_w_expert'][l]], axis=1)),
        br=np.concatenate([I['moe_b_group'][l], I['moe_b_expert'][l]])[None, :],
        mwg=I['moe_w_gate'][l], mwu=I['moe_w_up'][l], mwd=I['moe_w_down'][l],
        ident=_CONST['ident'], Rsel=np.repeat(np.eye(32, dtype=f), 128, axis=1),
    )
    common = {k: np.ascontiguousarray(v, dtype=f) for k, v in common.items()}
    maps = []
    for core in range(8):
        b, qt = core // 4, core % 4
        tok = slice(qt * NTOK, (qt + 1) * NTOK)
        d = dict(common)
        d['xT'] = np.ascontiguousarray(xT_all[b][:, :, tok])
        d['aT'] = _chunkT(attn[b][tok])
        d['sT'] = np.ascontiguousarray(ssmT[b][:, tok].reshape(4, 128, NTOK))
        d['cst'] = _col(I['c'][b]).astype(f)
        maps.append(d)
    res = run_bass_kernel_spmd(nc, maps, core_ids=list(range(8))).results
    x_new = [np.zeros((8, 128, SEQ), f) for _ in range(2)]
    x_nrm = [np.zeros((8, 128, SEQ), f) for _ in range(2)]
    for core in range(8):
        b, qt = core // 4, core % 4
        x_new[b][:, :, qt * NTOK:(qt + 1) * NTOK] = res[core]['o_x']
        x_nrm[b][:, :, qt * NTOK:(qt + 1) * NTOK] = res[core]['o_n']
    return x_new, x_nrm


def kernel(**inputs):
    I = {k: np.asarray(v, dtype=np.float32) for k, v in inputs.items()}
    xT_all = [_chunkT(I['x'][b]) for b in range(2)]
    x_nrm = None
    for l in range(2):
        attn, ssmT = _run_A(l, xT_all, I)
        xT_all, x_nrm = _run_B(l, xT_all, attn, ssmT, I)
    out = np.stack([x_nrm[b].reshape(1024, SEQ).T for b in range(2)])
    return np.ascontiguousarray(out, dtype=np.float32)
```
